# Optimizing a Trainium2 kernel written in Bass

```python
import math
import jax, jax.numpy as jnp
from jax import lax
import numpy as np

D_MODEL = 1024
BATCH = 8
SEQ = 2048
DEPTH = 1

CHUNK = 64
Q_BLOCK = 128
EPS = 1e-6
N_HEADS_A = 8
HEAD_DIM_A = 64
KV_DIM_A = 64
ATTN_OUT = N_HEADS_A * KV_DIM_A
N_IDX_HEADS = 4
IDX_DIM = 64
TOPK_MAX = 256
LRU_WIDTH = 512
LRU_BLOCKS = 8
LRU_BLOCK_DIM = LRU_WIDTH // LRU_BLOCKS
CONV_WIDTH = 4
LRU_C = 8.0
N_GROUPS = 4
EXPERTS_PER_GROUP = 8
N_EXPERTS = N_GROUPS * EXPERTS_PER_GROUP
TOPK_IN_GROUP = 2
D_FF_EXPERT = 256
SPLITS = (N_HEADS_A * HEAD_DIM_A, KV_DIM_A, KV_DIM_A, N_IDX_HEADS * IDX_DIM, IDX_DIM, N_IDX_HEADS,
          LRU_WIDTH, LRU_WIDTH, D_MODEL, D_MODEL)
D_IN = sum(SPLITS)

kernel_name = "hybrid_dsa_rglru_hmoe_block"


def _split_offsets():
    return [int(o) for o in np.cumsum(SPLITS)[:-1]]


def rmsnorm(x, g):
    xf = x.astype(jnp.float32)
    y = xf * lax.rsqrt(jnp.mean(xf * xf, axis=-1, keepdims=True) + EPS)
    return (y * g.astype(jnp.float32)).astype(x.dtype)


def modulate(h, shift, scale):
    return h * (1.0 + scale[:, None, :]) + shift[:, None, :]


def alibi_slopes(n):
    return jnp.exp2(-8.0 * jnp.arange(1, n + 1, dtype=jnp.float32) / n)


def dsa_attention(q, k, v, q_idx, k_idx, w_idx, q_norm_w, k_norm_w):
    B, S = q.shape[0], q.shape[1]
    topk = min(TOPK_MAX, S // 4)
    n_blocks = S // Q_BLOCK
    q = rmsnorm(q, q_norm_w) * (HEAD_DIM_A ** -0.5)
    k = rmsnorm(k, k_norm_w)
    w_idx = w_idx * (N_IDX_HEADS ** -0.5 * IDX_DIM ** -0.5)
    slopes = alibi_slopes(N_HEADS_A)
    key_chunk = jnp.arange(S) // CHUNK
    gather = jax.vmap(lambda table, idx: table[idx])

    def block(i):
        start = i * Q_BLOCK
        qb = lax.dynamic_slice_in_dim(q, start, Q_BLOCK, axis=1)
        qib = lax.dynamic_slice_in_dim(q_idx, start, Q_BLOCK, axis=1)
        wb = lax.dynamic_slice_in_dim(w_idx, start, Q_BLOCK, axis=1)
        q_pos = start + jnp.arange(Q_BLOCK)
        q_chunk = q_pos // CHUNK
        admissible = key_chunk[None, :] <= q_chunk[:, None]
        rel = jax.nn.relu(jnp.einsum('bqhd,bsd->bqhs', qib, k_idx).astype(jnp.float32))
        score = jnp.einsum('bqhs,bqh->bqs', rel, wb.astype(jnp.float32))
        score = jnp.where(admissible[None], score, -jnp.inf)
        _, sel = lax.top_k(score, topk)
        k_sel = gather(k, sel)
        v_sel = gather(v, sel)
        logits = jnp.einsum('bqhd,bqkd->bqhk', qb, k_sel).astype(jnp.float32)
        dist = jnp.abs(q_pos[None, :, None] - sel).astype(jnp.float32)
        logits = logits - slopes[None, None, :, None] * dist[:, :, None, :]
        valid = (sel // CHUNK) <= q_chunk[None, :, None]
        logits = jnp.where(valid[:, :, None, :], logits, -jnp.inf)
        p = jax.nn.softmax(logits, axis=-1).astype(v.dtype)
        return jnp.einsum('bqhk,bqkd->bqhd', p, v_sel)

    out = lax.map(block, jnp.arange(n_blocks))
    return jnp.moveaxis(out, 0, 1).reshape(B, S, ATTN_OUT)


def rg_lru_branch(xb, gb, conv_w, conv_b, w_rec_gate, b_rec_gate, w_in_gate, b_in_gate, lam):
    B, S, W = xb.shape
    xpad = jnp.pad(xb, ((0, 0), (CONV_WIDTH - 1, 0), (0, 0)))
    xc = conv_b + xpad[:, 0:S] * conv_w[0]
    for j in range(1, CONV_WIDTH):
        xc = xc + xpad[:, j:j + S] * conv_w[j]
    xblk = xc.reshape(B, S, LRU_BLOCKS, LRU_BLOCK_DIM)
    r = jax.nn.sigmoid(jnp.einsum('bsnd,nde->bsne', xblk, w_rec_gate).reshape(B, S, W) + b_rec_gate)
    i = jax.nn.sigmoid(jnp.einsum('bsnd,nde->bsne', xblk, w_in_gate).reshape(B, S, W) + b_in_gate)
    log_a = -LRU_C * r.astype(jnp.float32) * jax.nn.softplus(-lam.astype(jnp.float32))
    a = jnp.exp(log_a)
    b = jnp.sqrt(-jnp.expm1(2.0 * log_a)) * (i * xc).astype(jnp.float32)

    def combine(left, right):
        a1, b1 = left
        a2, b2 = right
        return a1 * a2, a2 * b1 + b2

    _, h = lax.associative_scan(combine, (a, b), axis=1)
    return h.astype(xb.dtype) * jax.nn.gelu(gb)


def hierarchical_moe(h, w_group, b_group, w_expert_router, b_expert_router, w1, w3, w2):
    B, S, D = h.shape
    hf = h.reshape(B * S, D)
    g_logits = (hf @ w_group + b_group).astype(jnp.float32)
    g_prob = jax.nn.softmax(g_logits, axis=-1)
    g_sel = jnp.argmax(g_logits, axis=-1)
    g_weight = jnp.take_along_axis(g_prob, g_sel[:, None], axis=1)
    e_logits = (hf @ w_expert_router + b_expert_router).astype(jnp.float32)
    e_logits = e_logits.reshape(-1, N_GROUPS, EXPERTS_PER_GROUP)
    e_logits = jnp.take_along_axis(e_logits, g_sel[:, None, None], axis=1)[:, 0]
    top_val, top_idx = lax.top_k(e_logits, TOPK_IN_GROUP)
    top_w = jax.nn.softmax(top_val, axis=-1) * g_weight
    expert_id = g_sel[:, None] * EXPERTS_PER_GROUP + top_idx
    comb = jnp.sum(jax.nn.one_hot(expert_id, N_EXPERTS, dtype=jnp.float32) * top_w[..., None], axis=1)
    comb = comb.astype(hf.dtype)
    y = jnp.zeros_like(hf)
    for e in range(N_EXPERTS):
        act = jax.nn.silu(hf @ w1[e]) * (hf @ w3[e])
        y = y + comb[:, e:e + 1] * (act @ w2[e])
    return y.reshape(B, S, D)


def setup_inputs(seed: int = 0) -> dict:
    key = jax.random.key(seed)
    ks = jax.random.split(key, 32)
    f32 = jnp.float32
    nrm = lambda k, shape, scale: jax.random.normal(k, shape, f32) * scale
    L, D = DEPTH, D_MODEL
    u = jax.random.uniform(ks[15], (L, LRU_WIDTH), f32, minval=0.9, maxval=0.999)
    sp = -jnp.log(u) / LRU_C
    lru_lambda = -jnp.log(jnp.expm1(sp))
    return {
        "x": nrm(ks[0], (BATCH, SEQ, D), 1.0),
        "c": nrm(ks[1], (BATCH, D), 1.0),
        "ada_w": nrm(ks[2], (L, D, 6 * D), 0.5 * D ** -0.5),
        "ada_b": nrm(ks[3], (L, 6 * D), 0.01),
        "norm_mix_w": 1.0 + nrm(ks[4], (L, D), 0.02),
        "w_in": nrm(ks[5], (L, D, D_IN), D ** -0.5),
        "q_norm_w": 1.0 + nrm(ks[6], (L, HEAD_DIM_A), 0.02),
        "k_norm_w": 1.0 + nrm(ks[7], (L, KV_DIM_A), 0.02),
        "conv_w": nrm(ks[8], (L, CONV_WIDTH, LRU_WIDTH), CONV_WIDTH ** -0.5),
        "conv_b": nrm(ks[9], (L, LRU_WIDTH), 0.01),
        "w_rec_gate": nrm(ks[10], (L, LRU_BLOCKS, LRU_BLOCK_DIM, LRU_BLOCK_DIM), LRU_BLOCK_DIM ** -0.5),
        "b_rec_gate": nrm(ks[11], (L, LRU_WIDTH), 0.01),
        "w_in_gate": nrm(ks[12], (L, LRU_BLOCKS, LRU_BLOCK_DIM, LRU_BLOCK_DIM), LRU_BLOCK_DIM ** -0.5),
        "b_in_gate": nrm(ks[13], (L, LRU_WIDTH), 0.01),
        "lru_lambda": lru_lambda,
        "w_proj_a": nrm(ks[16], (L, ATTN_OUT, D), ATTN_OUT ** -0.5),
        "w_proj_b": nrm(ks[17], (L, LRU_WIDTH, D), LRU_WIDTH ** -0.5),
        "w_out": nrm(ks[18], (L, D, D), D ** -0.5),
        "norm_ffn_w": 1.0 + nrm(ks[19], (L, D), 0.02),
        "w_group": nrm(ks[20], (L, D, N_GROUPS), D ** -0.5),
        "b_group": nrm(ks[21], (L, N_GROUPS), 0.01),
        "w_expert_router": nrm(ks[22], (L, D, N_EXPERTS), D ** -0.5),
        "b_expert_router": nrm(ks[23], (L, N_EXPERTS), 0.01),
        "w1": nrm(ks[24], (L, N_EXPERTS, D, D_FF_EXPERT), D ** -0.5),
        "w3": nrm(ks[25], (L, N_EXPERTS, D, D_FF_EXPERT), D ** -0.5),
        "w2": nrm(ks[26], (L, N_EXPERTS, D_FF_EXPERT, D), D_FF_EXPERT ** -0.5),
    }


def reference(x, c, ada_w, ada_b, norm_mix_w, w_in, q_norm_w, k_norm_w, conv_w, conv_b,
              w_rec_gate, b_rec_gate, w_in_gate, b_in_gate, lru_lambda, w_proj_a, w_proj_b,
              w_out, norm_ffn_w, w_group, b_group, w_expert_router, b_expert_router, w1, w3, w2):
    B, S, _ = x.shape
    cond = jax.nn.silu(c)
    offsets = _split_offsets()
    for l in range(DEPTH):
        mod = cond @ ada_w[l] + ada_b[l]
        shift_m, scale_m, gate_m, shift_f, scale_f, gate_f = jnp.split(mod, 6, axis=-1)
        h = modulate(rmsnorm(x, norm_mix_w[l]), shift_m, scale_m)
        proj = h @ w_in[l]
        q, k, v, q_idx, k_idx, w_idx, lru_x, lru_g, gate_a, gate_b = jnp.split(proj, offsets, axis=-1)
        q = q.reshape(B, S, N_HEADS_A, HEAD_DIM_A)
        q_idx = q_idx.reshape(B, S, N_IDX_HEADS, IDX_DIM)
        y_a = dsa_attention(q, k, v, q_idx, k_idx, w_idx, q_norm_w[l], k_norm_w[l]) @ w_proj_a[l]
        y_b = rg_lru_branch(lru_x, lru_g, conv_w[l], conv_b[l], w_rec_gate[l], b_rec_gate[l],
                            w_in_gate[l], b_in_gate[l], lru_lambda[l]) @ w_proj_b[l]
        merged = jax.nn.sigmoid(gate_a) * y_a + jax.nn.sigmoid(gate_b) * y_b
        x = x + gate_m[:, None, :] * (merged @ w_out[l])
        h2 = modulate(rmsnorm(x, norm_ffn_w[l]), shift_f, scale_f)
        x = x + gate_f[:, None, :] * hierarchical_moe(h2, w_group[l], b_group[l], w_expert_router[l],
                                                      b_expert_router[l], w1[l], w3[l], w2[l])
    return x
```

```python
import numpy as np
from contextlib import ExitStack
import concourse.bass as bass
import concourse.mybir as mybir
from concourse.bass_utils import run_bass_kernel_spmd
import ml_dtypes

F32 = mybir.dt.float32
BF16 = mybir.dt.bfloat16
ALU = mybir.AluOpType
AF = mybir.ActivationFunctionType
AX = mybir.AxisListType

D = 1024
SEQ = 2048
NT = 16
D_IN = 4036
EPS = 1e-6
NEXP = 32
NBIS = 12
ACT_BIS_FROM = 4
NCOL = 88
NROW = 36
NEG = -32768.0
SENT = -1.0e30


class Buf:
    __slots__ = ("name", "w", "r")

    def __init__(self, name):
        self.name = name
        self.w = None
        self.r = []


class Sched:
    def __init__(self, nc, es, ndma=16):
        self.nc = nc
        self.eng = {"pe": nc.tensor, "act": nc.scalar, "dve": nc.vector, "pool": nc.gpsimd, "sp": nc.sync}
        self.sem = {k: es.enter_context(nc.semaphore("s_" + k)) for k in self.eng}
        self.cnt = {k: 0 for k in self.eng}
        self.seen = {k: {} for k in self.eng}
        self.nd = ndma
        self.dsem = [es.enter_context(nc.semaphore(f"d{i}")) for i in range(2 * ndma)]
        self.dval = [0] * (2 * ndma)
        self.dnext = {"sp": 0, "pool": 0}
        self.bufs = {}
        self.dma_toks = []

    def B(self, *key):
        b = self.bufs.get(key)
        if b is None:
            b = self.bufs[key] = Buf(key)
        return b

    def _wait(self, e, tok):
        if tok is None:
            return
        kind, idx, val = tok
        if kind == "e" and idx == e and e == "pe":
            return
        key = (kind, idx)
        if self.seen[e].get(key, 0) >= val:
            return
        sem = self.sem[idx] if kind == "e" else self.dsem[idx]
        self.eng[e].wait_ge(sem, val)
        self.seen[e][key] = val

    def _deps(self, e, reads, writes):
        for b in reads:
            self._wait(e, b.w)
            if b.name[0] in ("bk", "PSU"):
                for t in b.r:
                    if t[1] != e:
                        self._wait(e, t)
        for b in writes:
            self._wait(e, b.w)
            for t in b.r:
                self._wait(e, t)

    def _commit(self, tok, reads, writes):
        for b in writes:
            b.w = tok
            b.r = []
        for b in reads:
            b.r.append(tok)

    def op(self, e, fn, reads=(), writes=()):
        self._deps(e, reads, writes)
        ins = fn(self.eng[e])
        self.cnt[e] += 1
        ins.then_inc(self.sem[e], 1)
        tok = ("e", e, self.cnt[e])
        self._commit(tok, reads, writes)
        return tok

    def dma(self, q, out, in_, reads=(), writes=()):
        i = self.dnext[q] + (0 if q == "sp" else self.nd)
        self.dnext[q] = (self.dnext[q] + 1) % self.nd
        if self.dval[i]:
            self._wait(q, ("d", i, self.dval[i]))
        self._deps(q, reads, writes)
        self.dval[i] += 16
        self.eng[q].dma_start(out=out, in_=in_).then_inc(self.dsem[i], 16)
        tok = ("d", i, self.dval[i])
        self._commit(tok, reads, writes)
        self.dma_toks.append(tok)
        return tok

    def barrier(self):
        engs = ["pe", "act", "dve", "pool"]
        for e in engs + ["sp"]:
            for o in engs:
                if self.cnt[o] and not (o == e == "pe"):
                    self._wait(e, ("e", o, self.cnt[o]))
            for i, v in enumerate(self.dval):
                if v:
                    self._wait(e, ("d", i, v))


class Arena:
    def __init__(self, nc, base, size, tag):
        self.nc, self.base, self.size, self.tag = nc, base, size, tag
        self.off = 0
        self.n = 0

    def alloc(self, name, shape, dt):
        nb = int(np.prod(shape[1:])) * (2 if dt == BF16 else 4)
        nb = (nb + 63) // 64 * 64
        assert self.off + nb <= self.size, (self.tag, name, self.off, nb, self.size)
        self.n += 1
        t = self.nc.alloc_sbuf_tensor_at(f"{self.tag}_{name}_{self.n}", list(shape), dt, offset=self.base + self.off)
        self.off += nb
        return t

    def reset(self):
        self.off = 0


def build_program(stop=None):
    nc = bass.Bass("TRN2", target_bir_lowering=False)
    with ExitStack() as es:
        _body(nc, es, stop)
    return nc


def _body(nc, es, stop):

    def din(name, shape, dt=F32):
        return nc.dram_tensor(name, list(shape), dt, kind="ExternalInput").ap()

    x_d = din("x", [SEQ, D])
    cols_d = din("cols", [128, NCOL])
    rows_d = din("rows", [128, NROW])
    gbias_d = din("gbias", [128, 2 * D])
    qkw_d = din("qkw", [128, 576])
    adaw_d = din("ada_w", [D, 6 * D])
    win_d = din("w_in", [D, D_IN])
    wpa_d = din("w_proj_a", [512, D])
    wpb_d = din("w_proj_b", [512, D])
    wout_d = din("w_out", [D, D])
    wrt_d = din("w_rt", [D, 36])
    wbd_d = din("wbd", [128, 2, 4, 128])
    w1_d = din("w1", [NEXP, D, 256])
    w3_d = din("w3", [NEXP, D, 256])
    w2_d = din("w2", [NEXP, 256, D])
    ktab_d = din("ktab", [3, SEQ], BF16)
    qtab_d = din("qtab", [3, 8, SEQ], BF16)
    dmat_d = din("dmat", [128, 8, 128], BF16)
    identb_d = din("identb", [128, 128], BF16)
    identf_d = din("identf", [128, 128])
    out_d = nc.dram_tensor("out", [SEQ, D], F32, kind="ExternalOutput").ap()
    dbg_d = {}

    win_v = win_d.rearrange("(kc p) n -> p kc n", p=128)
    adaw_v = adaw_d.rearrange("(kc p) n -> p kc n", p=128)

    S = Sched(nc, es)
    B = S.B
    V = lambda fn, r=(), w=(): S.op("dve", fn, r, w)
    A = lambda fn, r=(), w=(): S.op("act", fn, r, w)
    P = lambda fn, r=(), w=(): S.op("pool", fn, r, w)
    T = lambda fn, r=(), w=(): S.op("pe", fn, r, w)

    def finish(dumps):
        toks = []
        S.barrier()
        for name, ap, shape, dt in dumps:
            d = nc.dram_tensor("dbg_" + name, list(shape), dt, kind="ExternalOutput").ap()
            toks.append(S.dma("pool", d, ap))
        for t in toks:
            S._wait("pool", t)
        S.barrier()

    def staggered_g(make_gen, n, depth=2):
        active = []
        nxt = 0
        while nxt < n or active:
            if nxt < n and len(active) < depth:
                active.append(make_gen(nxt))
                nxt += 1
            for g in list(active):
                try:
                    next(g)
                except StopIteration:
                    active.remove(g)
            yield

    def staggered(make_gen, n, depth=2):
        for _ in staggered_g(make_gen, n, depth):
            pass

    def interleave(*gens):
        gens = [g for g in gens if g is not None]
        while gens:
            for g in list(gens):
                try:
                    next(g)
                except StopIteration:
                    gens.remove(g)

    BASE = 16512
    TOP = 229344
    G = Arena(nc, BASE, 13 * 1024, "G")
    R64 = Arena(nc, BASE + 13 * 1024, 64 * 1024, "R")
    M2 = Arena(nc, BASE + 77 * 1024, 57 * 1024, "M")
    L = Arena(nc, BASE + 134 * 1024, TOP - (BASE + 134 * 1024), "L")

    PS = [nc.alloc_psum_tensor(f"ps{i}", [128, 1024], F32) for i in range(4)]

    def bank(k):
        return PS[k // 2][:, (k % 2) * 512:(k % 2) * 512 + 512]

    def bankbf(k):
        return PS[k // 2][:, :].bitcast(BF16)[:, (k % 2) * 1024:(k % 2) * 1024 + 1024]

    identb = G.alloc("identb", [128, 128], BF16)
    identf = G.alloc("identf", [128, 128], F32)
    cols = G.alloc("cols", [128, NCOL], F32)
    rows = G.alloc("rows", [128, NROW], F32)
    ones_r = G.alloc("ones_r", [1, 128], F32)
    gate_m = G.alloc("gate_m", [128, D], F32)
    gate_f = G.alloc("gate_f", [128, D], F32)
    sm = G.alloc("sm", [128, 256], F32)
    sm2 = G.alloc("sm2", [128, 64], F32)
    modp = G.alloc("modp", [128, 32], F32)
    dgt = G.alloc("dgt", [128, 128], F32)
    cond_bc = nc.alloc_sbuf_tensor_at("cond_bc", [128, 8, 128], F32, offset=TOP - 4096 - 64)
    COND = sm[:, 0:8]
    MODC = sm[:, 8:40]
    A_M = sm[:, 40:48]
    B_M = sm[:, 8:16]
    A_F = sm[:, 48:56]
    B_F = sm[:, 24:32]
    SS = sm[:, 56:72]
    RSTD = sm[:, 72:88]
    VAR = sm[:, 88:104]
    NHALF = sm[:, 104:105]
    NH16 = sm[:, 228:244]
    CA = sm[:, 105:109]
    SPT = sm[:, 109:113]
    SSQ = sm[:, 113:122]
    RQ = sm[:, 122:131]
    VQ = sm[:, 131:140]
    HBT = sm[:, 140:144]
    HBI = sm[:, 144:148]
    BIS = sm2[:, 0:48]
    SS2 = sm[:, 180:196]
    RSTD2 = sm[:, 196:212]
    VAR2 = sm[:, 212:228]

    c_cond = B("cond")
    c_sm = B("sm")

    S.dma("pool", cols[:], cols_d, writes=[B("cols")])
    S.dma("pool", rows[:], rows_d, writes=[B("rows")])
    S.dma("pool", gate_m[:], gbias_d[:, 0:D], writes=[B("gate", 2)])
    S.dma("pool", gate_f[:], gbias_d[:, D:2 * D], writes=[B("gate", 5)])
    S.dma("pool", identb[:], identb_d, writes=[B("identb")])
    S.dma("pool", identf[:], identf_d, writes=[B("identf")])
    V(lambda e: e.memset(sm[:], 0.0), w=[c_sm])
    V(lambda e: e.memset(sm2[:], 0.0), w=[c_sm])
    S.barrier()
    V(lambda e: e.memset(ones_r[:], 1.0), w=[B("ones_r")])
    V(lambda e: e.memset(NHALF, -0.5), w=[B("nhalf")])
    V(lambda e: e.memset(NH16, -0.5), w=[B("nhalf")])

    A(lambda e: e.activation(out=COND, in_=cols[:, 0:8], func=AF.Silu), r=[B("cols")], w=[c_cond])
    V(lambda e: e.tensor_copy(out=cond_bc[:], in_=COND[:, :, None].to_broadcast([128, 8, 128])), r=[c_cond], w=[B("cond_bc")])

    hT = R64.alloc("hT", [128, 8, SEQ], BF16)
    hgT = R64.alloc("hgT", [128, 4, SEQ], BF16)
    attnT = R64.alloc("attnT", [128, 4, SEQ], BF16)
    ada_st = [nc.alloc_sbuf_tensor_at("ada_st0", [128, 8, 512], F32, offset=BASE + 13 * 1024 + 48 * 1024),
              nc.alloc_sbuf_tensor_at("ada_st1", [128, 8, 512], F32, offset=BASE + 13 * 1024 + 32 * 1024)]

    ada_n = [0]

    ada_slot = {}

    def ada_dma(pidx, slot=None):
        if slot is None:
            slot = ada_n[0] % 2
            ada_n[0] += 1
        ada_slot[pidx] = slot
        S.dma("sp", ada_st[slot][:], adaw_v[:, :, pidx * 512:(pidx + 1) * 512], writes=[B("ada_st", slot)])

    def ada_mm(pidx, kb=None):
        slot = ada_slot[pidx]
        st = ada_st[slot]
        bst = B("ada_st", slot)
        vec = pidx // 2
        half = pidx % 2
        if vec in (2, 5):
            kg_ = 1 if kb is None else kb
            pb = bank(kg_)
            for kc in range(8):
                T(lambda e, kc=kc: e.matmul(pb, lhsT=cond_bc[:, kc, :], rhs=st[:, kc, :], start=(kc == 0), stop=(kc == 7)),
                  r=[bst, B("cond_bc")], w=[B("bk", kg_)])
            dst = gate_m if vec == 2 else gate_f
            V(lambda e: e.tensor_tensor(out=dst[:, half * 512:(half + 1) * 512], in0=pb, in1=dst[:, half * 512:(half + 1) * 512], op=ALU.add),
              r=[B("bk", kg_), B("gate", vec)], w=[B("gate", vec)])
        else:
            vi = {0: 0, 1: 1, 3: 2, 4: 3}[vec]
            kg_ = 1 if kb is None else kb
            pb = bank(kg_)
            for kc in range(8):
                T(lambda e, kc=kc: e.matmul(pb, lhsT=cond_bc[:, kc, :], rhs=st[:, kc, :], start=(kc == 0), stop=(kc == 7)),
                  r=[bst, B("cond_bc")], w=[B("bk", kg_)])
            for nn in range(4):
                col = vi * 8 + half * 4 + nn
                V(lambda e, nn=nn: e.tensor_tensor(out=dgt[:], in0=pb[:, nn * 128:(nn + 1) * 128], in1=identf[:], op=ALU.mult), r=[B("bk", kg_), B("identf")], w=[B("dgt")])
                V(lambda e, col=col: e.tensor_reduce(out=modp[:, col:col + 1], in_=dgt[:], axis=AX.X, op=ALU.add), r=[B("dgt")], w=[B("modp")])

    def ada_piece(pidx):
        ada_dma(pidx)
        ada_mm(pidx)

    def ada_finish(which):
        lo = 0 if which == 0 else 16
        cb = 24 if which == 0 else 40
        V(lambda e: e.tensor_tensor(out=MODC[:, lo:lo + 16], in0=modp[:, lo:lo + 16], in1=cols[:, cb:cb + 16], op=ALU.add),
          r=[B("modp"), B("cols")], w=[c_sm])
        nw = cols[:, 8:16] if which == 0 else cols[:, 16:24]
        dst = A_M if which == 0 else A_F
        V(lambda e: e.scalar_tensor_tensor(out=dst, in0=MODC[:, lo + 8:lo + 16], scalar=1.0, in1=nw, op0=ALU.add, op1=ALU.mult),
          r=[c_sm, B("cols")], w=[B("AB", which)])

    for p_ in range(4):
        ada_piece(p_)
    ada_finish(0)

    qTa = M2.alloc("qTa", [128, 8, SEQ], BF16)
    kTa = M2.alloc("kTa", [128, SEQ], BF16)
    v_aug = M2.alloc("v_aug", [128, NT, 66], BF16)
    qiT = M2.alloc("qiT", [128, 2, SEQ], BF16)
    kiT = M2.alloc("kiT", [128, 2, SEQ], BF16)
    widx = M2.alloc("widx", [128, NT, 4], F32)
    dmat = M2.alloc("dmat", [128, 8, 128], BF16)
    S.dma("pool", qTa[64:67, :, :], qtab_d, writes=[B("qTa_aug")])
    S.dma("pool", kTa[64:67, :], ktab_d, writes=[B("kTa_aug")])
    S.dma("pool", dmat[:], dmat_d, writes=[B("dmat")])
    P(lambda e: e.memset(v_aug[:, :, 64:66], 1.0), w=[B("v_ones")])

    wst = [L.alloc(f"wst{i}", [128, 8, 256], F32) for i in range(2)]
    wfm = [L.alloc(f"wfm{i}", [128, 8, 128], BF16) for i in range(4)]
    wbd_b = L.alloc("wbd_b", [128, 2, 4, 128], BF16)
    Lmark = L.off
    Wtm = L.alloc("Wtm", [128, 8, 644], BF16)
    wbd_f = L.alloc("wbd_f", [128, 2, 4, 128], F32)
    qkw = L.alloc("qkw", [128, 576], F32)
    xin = [L.alloc(f"xin{i}", [128, D], F32) for i in range(2)]
    xn = [L.alloc(f"xn{i}", [128, D], BF16) for i in range(2)]
    sqj = L.alloc("sqj", [128, D], BF16)
    qk2 = [L.alloc(f"qk{i}", [128, 576], F32) for i in range(2)]
    sq2 = [L.alloc(f"sq{i}", [128, 576], F32) for i in range(2)]
    qkn2 = [L.alloc(f"qkn{i}", [128, 576], BF16) for i in range(2)]

    S.dma("pool", wbd_f[:], wbd_d, writes=[B("wbd_f")])
    S.dma("pool", qkw[:], qkw_d, writes=[B("qkw")])
    P(lambda e: e.tensor_copy(out=wbd_b[:], in_=wbd_f[:]), r=[B("wbd_f")], w=[B("wbd_b")])
    P(lambda e: e.tensor_scalar(out=qkw[:, 0:512], in0=qkw[:, 0:512], scalar1=0.125, scalar2=None, op0=ALU.mult), r=[B("qkw")], w=[B("qkw")])

    wst_n = [0]

    def load_cast(dst_ap, bdst, c0, w, eng="pool", dram_v=None):
        slot = wst_n[0] % 2
        wst_n[0] += 1
        st = wst[slot]
        bst = B("wst", slot)
        src = (win_v if dram_v is None else dram_v)[:, :, c0:c0 + w]
        nk = src.shape[1]
        S.dma("sp", st[:, 0:nk, 0:w], src, writes=[bst])
        S.op(eng, lambda e: e.tensor_copy(out=dst_ap, in_=st[:, 0:nk, 0:w]), [bst], [bdst])

    bWtm = B("Wtm")
    load_cast(Wtm[:, :, 0:256], bWtm, 0, 256)
    load_cast(Wtm[:, :, 256:512], bWtm, 256, 256)
    load_cast(Wtm[:, :, 512:640], bWtm, 512, 128)
    load_cast(Wtm[:, :, 640:644], bWtm, 960, 4)

    def a1_tile(i):
        sl = i % 2
        bx = B("xin", sl)
        S.dma("pool", xin[sl][:], x_d[i * 128:(i + 1) * 128, :], writes=[bx])
        A(lambda e: e.activation(out=sqj[:], in_=xin[sl][:], func=AF.Square, accum_out=SS[:, i:i + 1]),
          r=[bx], w=[B("sqj"), B("ss", i)])
        yield
        V(lambda e: e.tensor_scalar(out=VAR[:, i:i + 1], in0=SS[:, i:i + 1], scalar1=1.0 / D, scalar2=EPS, op0=ALU.mult, op1=ALU.add),
          r=[B("ss", i)], w=[B("var", i)])
        yield
        P(lambda e: e.tensor_tensor(out=RSTD[:, i:i + 1], in0=VAR[:, i:i + 1], in1=NHALF, op=ALU.pow),
          r=[B("var", i), B("nhalf")], w=[B("rstd", i)])
        yield
        V(lambda e: e.tensor_scalar(out=xn[sl][:], in0=xin[sl][:], scalar1=RSTD[:, i:i + 1], scalar2=None, op0=ALU.mult),
          r=[bx, B("rstd", i)], w=[B("xn", sl)])
        yield
        bk = sl
        tp = bankbf(bk).rearrange("p (a b) -> p a b", b=128)
        for dc in range(8):
            T(lambda e, dc=dc: e.transpose(tp[:, dc, :], xn[sl][:, dc * 128:(dc + 1) * 128], identb[:]),
              r=[B("xn", sl), B("identb")], w=[B("bk", bk)])
        yield
        for dc in range(8):
            o = hT[:, dc, i * 128:(i + 1) * 128]
            if sl == 0:
                A(lambda e, dc=dc, o=o: e.activation(out=o, in_=tp[:, dc, :], func=AF.Identity, scale=A_M[:, dc:dc + 1], bias=B_M[:, dc:dc + 1]),
                  r=[B("bk", bk), B("AB", 0), c_sm], w=[B("hT", i)])
            else:
                V(lambda e, dc=dc, o=o: e.tensor_scalar(out=o, in0=tp[:, dc, :], scalar1=A_M[:, dc:dc + 1], scalar2=B_M[:, dc:dc + 1], op0=ALU.mult, op1=ALU.add),
                  r=[B("bk", bk), B("AB", 0), c_sm], w=[B("hT", i)])
            if dc % 4 == 3:
                yield

    if stop == "A1":
        staggered(a1_tile, NT)
    if stop == "A1":
        finish([("hT", hT[:], [128, 8, SEQ], BF16), ("sm", sm[:], [128, 256], F32)])
        return

    if stop == "A1b":
        finish([("gate_m", gate_m[:], [128, D], F32), ("gate_f", gate_f[:], [128, D], F32), ("sm", sm[:], [128, 256], F32)])
        return
    def a2_tile(i):
        st_ = i % 2
        kq, kk, ktq, ktk = (4, 5, 6, 5) if st_ == 0 else (7, 2, 3, 2)
        qk_, sq_, qkn_ = qk2[st_], sq2[st_], qkn2[st_]
        SSQ_, RQ_, VQ_ = sm2[:, st_ * 32:st_ * 32 + 9], sm2[:, st_ * 32 + 9:st_ * 32 + 18], sm2[:, st_ * 32 + 18:st_ * 32 + 27]
        bqk, bsq, bqkn, bv_ = B("qk", st_), B("sq", st_), B("qkn", st_), B("vq", st_)
        bh = B("hT", i)
        for dc in range(8):
            T(lambda e, dc=dc: e.matmul(bank(kq), lhsT=hT[:, dc, i * 128:(i + 1) * 128], rhs=Wtm[:, dc, 0:512], start=(dc == 0), stop=(dc == 7)),
              r=[bh, bWtm], w=[B("bk", kq)])
        for dc in range(8):
            T(lambda e, dc=dc: e.matmul(bank(kk)[:, 0:132], lhsT=hT[:, dc, i * 128:(i + 1) * 128], rhs=Wtm[:, dc, 512:644], start=(dc == 0), stop=(dc == 7)),
              r=[bh, bWtm], w=[B("bk", kk)])
        yield
        A(lambda e: e.activation(out=qk_[:, 0:512], in_=bank(kq), func=AF.Copy), r=[B("bk", kq)], w=[bqk])
        V(lambda e: e.tensor_copy(out=qk_[:, 512:576], in_=bank(kk)[:, 0:64]), r=[B("bk", kk)], w=[bqk])
        V(lambda e: e.tensor_copy(out=v_aug[:, i, 0:64], in_=bank(kk)[:, 64:128]), r=[B("bk", kk)], w=[B("v", i)])
        V(lambda e: e.tensor_copy(out=widx[:, i, :], in_=bank(kk)[:, 128:132]), r=[B("bk", kk)], w=[B("widx", i)])
        yield
        V(lambda e: e.tensor_tensor(out=sq_[:], in0=qk_[:], in1=qk_[:], op=ALU.mult), r=[bqk], w=[bsq])
        V(lambda e: e.tensor_reduce(out=SSQ_, in_=sq_[:].rearrange("p (a b) -> p a b", b=64), axis=AX.X, op=ALU.add), r=[bsq], w=[bv_])
        V(lambda e: e.tensor_scalar(out=VQ_, in0=SSQ_, scalar1=1.0 / 64, scalar2=EPS, op0=ALU.mult, op1=ALU.add), r=[bv_], w=[bv_])
        yield
        P(lambda e: e.tensor_tensor(out=RQ_, in0=VQ_, in1=NH16[:, 0:9], op=ALU.pow), r=[bv_, B("nhalf")], w=[bv_])
        yield
        V(lambda e: e.tensor_tensor(out=qk_[:].rearrange("p (a b) -> p a b", b=64), in0=qk_[:].rearrange("p (a b) -> p a b", b=64),
                                    in1=RQ_[:, :, None].to_broadcast([128, 9, 64]), op=ALU.mult), r=[bqk, bv_], w=[bqk])
        V(lambda e: e.tensor_tensor(out=qkn_[:], in0=qk_[:], in1=qkw[:], op=ALU.mult), r=[bqk, B("qkw")], w=[bqkn])
        yield
        tq = bankbf(ktq).rearrange("p (a b) -> p a b", b=128)
        tk = bankbf(ktk)[:, 512:640]
        for h in range(8):
            T(lambda e, h=h: e.transpose(tq[0:64, h, :], qkn_[:, h * 64:(h + 1) * 64], identb[:]), r=[bqkn, B("identb")], w=[B("bk", ktq)])
        T(lambda e: e.transpose(tk[0:64, 0:128], qkn_[:, 512:576], identb[:]), r=[bqkn, B("identb")], w=[B("bk", ktk)])
        yield
        A(lambda e: e.activation(out=qTa[0:64, :, i * 128:(i + 1) * 128], in_=tq[0:64, :, :], func=AF.Copy), r=[B("bk", ktq)], w=[B("qT", i)])
        V(lambda e: e.tensor_copy(out=kTa[0:64, i * 128:(i + 1) * 128], in_=tk[0:64, 0:128]), r=[B("bk", ktk)], w=[B("kT", i)])
        yield

    fm_n = [0]
    bk_n = [0]

    def next_bank():
        k = bk_n[0] % 8
        bk_n[0] += 1
        return k

    def fm_weights(c0, w, place=0, zero=False, slot=None):
        if slot is None:
            slot = fm_n[0] % 4
            fm_n[0] += 1
        bw = B("wfm", slot)
        if zero:
            P(lambda e: e.memset(wfm[slot][:], 0.0), w=[bw])
        load_cast(wfm[slot][:, :, place:place + w], bw, c0, w)
        return slot

    def fm_mm(slot, tb, k):
        for dc in range(8):
            T(lambda e, dc=dc: e.matmul(bank(k), lhsT=wfm[slot][:, dc, :], rhs=hT[:, dc, tb * 512:(tb + 1) * 512], start=(dc == 0), stop=(dc == 7)),
              r=[B("wfm", slot)] + [B("hT", 4 * tb + j) for j in range(4)], w=[B("bk", k)])

    kq_slots = [fm_weights(896, 64, place=0, zero=True), fm_weights(896, 64, place=64, zero=True),
                fm_weights(640, 128), fm_weights(768, 128)]

    def kiqi_block(tb):
        for u_, slot in enumerate(kq_slots):
            k = (1, 7, 2, 3)[u_]
            fm_mm(slot, tb, k)
            yield
            if u_ < 2:
                o = kiT[:, u_, tb * 512:(tb + 1) * 512]
                bo = B("kiT", tb)
            else:
                o = qiT[:, u_ - 2, tb * 512:(tb + 1) * 512]
                bo = B("qiT", tb)
            if u_ % 2 == 0:
                A(lambda e, o=o, k=k: e.activation(out=o, in_=bank(k), func=AF.Copy), r=[B("bk", k)], w=[bo])
            else:
                V(lambda e, o=o, k=k: e.tensor_copy(out=o, in_=bank(k)), r=[B("bk", k)], w=[bo])
            yield

    def a12_tile(i):
        yield from a1_tile(i)
        yield from a2_tile(i)
        if i % 4 == 3:
            yield from kiqi_block(i // 4)

    staggered(a12_tile, NT, depth=2)
    if stop == "A2":
        finish([("qTa", qTa[0:67], [67, 8, SEQ], BF16), ("kTa", kTa[0:67], [67, SEQ], BF16), ("v_aug", v_aug[:], [128, NT, 66], BF16),
                ("widx", widx[:], [128, NT, 4], F32), ("sm", sm[:], [128, 256], F32)])
        return

    S.barrier()
    L.off = Lmark
    wfm.append(L.alloc("wfm4", [128, 8, 128], BF16))
    wfm.append(L.alloc("wfm5", [128, 8, 128], BF16))
    lset = []
    for st_ in range(2):
        d_ = dict(xraw=L.alloc(f"xraw{st_}", [128, 516], F32), xc=L.alloc(f"xc{st_}", [128, 512], F32), xcb=L.alloc(f"xcb{st_}", [128, 512], BF16),
                  t_r=L.alloc(f"t_r{st_}", [128, 512], F32), t_i=L.alloc(f"t_i{st_}", [128, 512], F32),
                  om=L.alloc(f"om{st_}", [128, 512], F32), hb=[L.alloc(f"hb{st_}_{i}", [128, 512], F32) for i in range(2)],
                  g_t=L.alloc(f"g_t{st_}", [128, 512], F32), u_t=L.alloc(f"u_t{st_}", [128, 512], F32))
        lset.append(d_)
    A(lambda e: e.activation(out=SPT, in_=cols[:, 84:88], func=AF.Exp, scale=-1.0), r=[B("cols")], w=[B("spt")])
    A(lambda e: e.activation(out=SPT, in_=SPT, func=AF.Ln, bias=1.0), r=[B("spt")], w=[B("spt")])
    V(lambda e: e.tensor_scalar(out=CA, in0=SPT, scalar1=-4.0, scalar2=None, op0=ALU.mult), r=[B("spt")], w=[B("ca")])
    V(lambda e: e.tensor_scalar(out=HBT, in0=cols[:, 76:80], scalar1=0.5, scalar2=None, op0=ALU.mult), r=[B("cols")], w=[B("hbt")])
    V(lambda e: e.tensor_scalar(out=HBI, in0=cols[:, 80:84], scalar1=0.5, scalar2=None, op0=ALU.mult), r=[B("cols")], w=[B("hbi")])

    lru_wdone = set()

    def lru_chunk(c):
        st_ = c % 2
        d_ = lset[st_]
        xraw, xc, xcb, t_r, t_i, om, hb, g_t, u_t = (d_[k] for k in ("xraw", "xc", "xcb", "t_r", "t_i", "om", "hb", "g_t", "u_t"))
        gg = g_t
        ix = t_i
        a_t = t_r
        nm = lambda k: B(k, st_)
        def lru_w(cc):
            if cc not in lru_wdone and cc < 4:
                lru_wdone.add(cc)
                pr = cc % 3
                fm_weights(964 + 128 * cc, 128, slot=2 * pr)
                fm_weights(1476 + 128 * cc, 128, slot=2 * pr + 1)
        lru_w(c)
        lru_w(c + 1)
        sx, sg = 2 * (c % 3), 2 * (c % 3) + 1
        V(lambda e: e.memset(xraw[:, 0:3], 0.0), w=[nm("xraw")])
        yield
        for tb in range(4):
            kx, kg, kr, ki = next_bank(), next_bank(), next_bank(), next_bank()
            fm_mm(sx, tb, kx)
            yield
            fm_mm(sg, tb, kg)
            yield
            A(lambda e: e.activation(out=xraw[:, 3:515], in_=bank(kx), func=AF.Copy), r=[B("bk", kx)], w=[nm("xraw")])
            A(lambda e: e.activation(out=g_t[:], in_=bank(kg), func=AF.Copy), r=[B("bk", kg)], w=[nm("g_t")])
            yield
            V(lambda e: e.tensor_scalar(out=xc[:], in0=xraw[:, 0:512], scalar1=cols[:, 56 + c:57 + c], scalar2=cols[:, 72 + c:73 + c], op0=ALU.mult, op1=ALU.add),
              r=[nm("xraw"), B("cols")], w=[nm("xc")])
            for j in range(1, 4):
                V(lambda e, j=j: e.scalar_tensor_tensor(out=xc[:], in0=xraw[:, j:j + 512], scalar=cols[:, 56 + 4 * j + c:57 + 4 * j + c], in1=xc[:], op0=ALU.mult, op1=ALU.add),
                  r=[nm("xraw"), B("cols"), nm("xc")], w=[nm("xc")])
            V(lambda e: e.tensor_copy(out=xraw[:, 0:3], in_=xraw[:, 512:515]), r=[nm("xraw")], w=[nm("xraw")])
            yield
            P(lambda e: e.tensor_copy(out=xcb[:], in_=xc[:]), r=[nm("xc")], w=[nm("xcb")])
            P(lambda e: e.tensor_tensor(out=u_t[:], in0=g_t[:], in1=g_t[:], op=ALU.mult), r=[nm("g_t")], w=[nm("u_t")])
            yield
            T(lambda e: e.matmul(bank(kr), lhsT=wbd_b[:, 0, c, :], rhs=xcb[:], start=True, stop=True), r=[B("wbd_b"), nm("xcb")], w=[B("bk", kr)])
            T(lambda e: e.matmul(bank(ki), lhsT=wbd_b[:, 1, c, :], rhs=xcb[:], start=True, stop=True), r=[B("wbd_b"), nm("xcb")], w=[B("bk", ki)])
            V(lambda e: e.tensor_scalar(out=u_t[:], in0=u_t[:], scalar1=0.044715, scalar2=1.0, op0=ALU.mult, op1=ALU.add), r=[nm("u_t")], w=[nm("u_t")])
            yield
            A(lambda e: e.activation(out=t_r[:], in_=bank(kr), func=AF.Tanh, scale=0.5, bias=HBT[:, c:c + 1]), r=[B("bk", kr), B("hbt")], w=[nm("t_r")])
            A(lambda e: e.activation(out=t_i[:], in_=bank(ki), func=AF.Tanh, scale=0.5, bias=HBI[:, c:c + 1]), r=[B("bk", ki), B("hbi")], w=[nm("t_i")])
            P(lambda e: e.tensor_tensor(out=u_t[:], in0=u_t[:], in1=g_t[:], op=ALU.mult), r=[nm("u_t"), nm("g_t")], w=[nm("u_t")])
            yield
            A(lambda e: e.activation(out=a_t[:], in_=t_r[:], func=AF.Exp, scale=CA[:, c:c + 1], bias=CA[:, c:c + 1]), r=[nm("t_r"), B("ca")], w=[nm("t_r")])
            A(lambda e: e.activation(out=u_t[:], in_=u_t[:], func=AF.Tanh, scale=0.7978845608028654), r=[nm("u_t")], w=[nm("u_t")])
            V(lambda e: e.scalar_tensor_tensor(out=ix[:], in0=t_i[:], scalar=1.0, in1=xc[:], op0=ALU.add, op1=ALU.mult), r=[nm("t_i"), nm("xc")], w=[nm("t_i")])
            yield
            P(lambda e: e.tensor_tensor(out=om[:], in0=a_t[:], in1=a_t[:], op=ALU.mult), r=[nm("t_r")], w=[nm("om")])
            V(lambda e: e.scalar_tensor_tensor(out=gg[:], in0=u_t[:], scalar=1.0, in1=g_t[:], op0=ALU.add, op1=ALU.mult), r=[nm("u_t"), nm("g_t")], w=[nm("g_t")])
            yield
            A(lambda e: e.activation(out=om[:], in_=om[:], func=AF.Sqrt, scale=-1.0, bias=1.0), r=[nm("om")], w=[nm("om")])
            yield
            P(lambda e: e.tensor_tensor(out=ix[:], in0=ix[:], in1=om[:], op=ALU.mult), r=[nm("t_i"), nm("om")], w=[nm("t_i")])
            yield
            hs = tb % 2
            init = 0.0 if tb == 0 else hb[1 - hs][:, 511:512]
            V(lambda e, hs=hs, init=init: e.tensor_tensor_scan(out=hb[hs][:], data0=a_t[:], data1=ix[:], initial=init, op0=ALU.mult, op1=ALU.add),
              r=[nm("t_r"), nm("t_i"), B("hb", st_, 1 - hs)], w=[B("hb", st_, hs)])
            yield
            V(lambda e, hs=hs: e.scalar_tensor_tensor(out=hgT[:, c, tb * 512:(tb + 1) * 512], in0=hb[hs][:], scalar=0.25, in1=gg[:], op0=ALU.mult, op1=ALU.mult),
              r=[B("hb", st_, hs), nm("g_t")], w=[B("hgT", tb)])
            yield

    def ada_bg():
        for p_ in range(4, 12):
            ada_dma(p_, slot=0)
            for _ in range(12):
                yield
            ada_mm(p_, kb=next_bank())
            yield
        ada_finish(1)

    interleave(staggered_g(lru_chunk, 4), ada_bg())

    S.barrier()
    if stop == "A3":
        finish([("qiT", qiT[:], [128, 2, SEQ], BF16), ("kiT", kiT[:], [128, 2, SEQ], BF16), ("hgT", hgT[:], [128, 4, SEQ], BF16)])
        return

    L.reset()
    score = [L.alloc(f"score{i}", [128, SEQ], F32) for i in range(3)]
    mb = [L.alloc(f"mb{i}", [128, SEQ], BF16) for i in range(2)]
    junk = L.alloc("junk", [128, SEQ], BF16)
    rtmp = [L.alloc(f"rtmp{i}", [128, 256], F32) for i in range(4)]
    gehi = L.alloc("gehi", [128, SEQ], BF16)
    junkA = L.alloc("junkA", [128, SEQ], BF16)
    bandb = L.alloc("bandb", [128, SEQ], BF16)
    nstp = L.alloc("nstp", [128, 3, NBIS], F32)
    cum = L.alloc("cum", [128, SEQ], F32)
    onesb = L.alloc("onesb", [128, SEQ], BF16)
    V(lambda e: e.memset(onesb[:], 1.0), w=[B("onesb")])
    PT = [L.alloc(f"PT{i}", [128, 512], BF16) for i in range(3)]
    attn_tm = L.alloc("attn_tm", [128, 512], BF16)
    rs = L.alloc("rs", [128, 8], F32)
    stp = L.alloc("stp", [128, 3, NBIS], F32)
    cvec = L.alloc("cvec", [128, NBIS], F32)
    for k in range(NBIS):
        V(lambda e, k=k: e.memset(cvec[:, k:k + 1], 2.0 ** -(k + 1)), w=[B("cvec")])

    def _hdr(i):
        par = i % 3
        Lk = 128 * (i + 1)
        sc = score[i % 3]
        bs = B("score", i % 3)
        LO = BIS[:, par * 16 + 0:par * 16 + 1]
        W0 = BIS[:, par * 16 + 1:par * 16 + 2]
        MID = BIS[:, par * 16 + 2:par * 16 + 3]
        CNT = BIS[:, par * 16 + 3:par * 16 + 4]
        FS = BIS[:, par * 16 + 4:par * 16 + 5]
        AM = BIS[:, par * 16 + 5:par * 16 + 6]
        bb = B("bis", par)
        return par, Lk, sc, bs, LO, W0, MID, CNT, FS, AM, bb

    def idx_scores(i):
        par, Lk, sc, bs, LO, W0, MID, CNT, FS, AM, bb = _hdr(i)
        if i < 2:
            V(lambda e: e.memset(sc[:, 0:Lk], 0.0), w=[bs])
            V(lambda e: e.memset(sc[0:64, Lk - 64:Lk], SENT), w=[bs])
            V(lambda e: e.memset(LO, -1.0), w=[bb])
            yield
        else:
            nblk = (Lk + 255) // 256
            for h in range(4):
                for bl in range(nblk):
                    half = bl % 2
                    c0 = bl * 256
                    cw_ = min(256, Lk - c0)
                    pidx = bank(half)[:, 0:cw_]
                    T(lambda e, h=h, c0=c0, cw_=cw_, pidx=pidx: e.matmul(pidx, lhsT=qiT[:, h // 2, i * 128:(i + 1) * 128], rhs=kiT[:, h % 2, c0:c0 + cw_], start=True, stop=True),
                      r=[B("qiT", i // 4), B("kiT", bl // 2)], w=[B("bk", half)])
                    rt = rtmp[(h * nblk + bl) % 4]
                    brt = B("rtmp", (h * nblk + bl) % 4)
                    dst = sc[:, c0:c0 + cw_]
                    if h == 0:
                        V(lambda e, pidx=pidx, dst=dst: e.tensor_scalar(out=dst, in0=pidx, scalar1=0.0, scalar2=widx[:, i, 0:1], op0=ALU.max, op1=ALU.mult),
                          r=[B("bk", half), B("widx", i)], w=[bs])
                    else:
                        V(lambda e, pidx=pidx, rt=rt, h=h, cw_=cw_: e.tensor_scalar(out=rt[:, 0:cw_], in0=pidx, scalar1=0.0, scalar2=widx[:, i, h:h + 1], op0=ALU.max, op1=ALU.mult),
                          r=[B("bk", half), B("widx", i)], w=[brt])
                        P(lambda e, rt=rt, dst=dst, cw_=cw_: e.tensor_tensor(out=dst, in0=dst, in1=rt[:, 0:cw_], op=ALU.add), r=[brt, bs], w=[bs])
                    yield
            V(lambda e: e.tensor_reduce(out=AM, in_=sc[:, 0:Lk], axis=AX.X, op=ALU.max, apply_absolute_value=True), r=[bs], w=[bb])
            V(lambda e: e.tensor_scalar(out=LO, in0=AM, scalar1=-1.001, scalar2=-1e-20, op0=ALU.mult, op1=ALU.add), r=[bb], w=[bb])
            V(lambda e: e.tensor_scalar(out=W0, in0=LO, scalar1=-2.0, scalar2=None, op0=ALU.mult), r=[bb], w=[bb])
            V(lambda e: e.tensor_tensor(out=stp[:, par, :], in0=cvec[:], in1=W0.to_broadcast([128, NBIS]), op=ALU.mult), r=[bb, B("cvec")], w=[B("stp", par)])
            V(lambda e: e.memset(sc[0:64, Lk - 64:Lk], SENT), r=[bs], w=[bs])
            yield

    def bisect(i):
        par, Lk, sc, bs, LO, W0, MID, CNT, FS, AM, bb = _hdr(i)
        if i >= 2:
            SL = stp[:, par, NBIS - 1:NBIS]
            yield
            if i < ACT_BIS_FROM:
                V(lambda e: e.tensor_tensor(out=MID, in0=LO, in1=stp[:, par, 0:1], op=ALU.add), r=[bb, B("stp", par)], w=[bb])
                for k in range(NBIS):
                    V(lambda e: e.tensor_scalar(out=junk[:, 0:Lk], in0=sc[:, 0:Lk], scalar1=MID, scalar2=None, op0=ALU.is_ge, op1=ALU.add, accum_out=CNT),
                      r=[bs, bb], w=[B("junk"), bb])
                    V(lambda e: e.tensor_scalar(out=FS, in0=CNT, scalar1=255.5, scalar2=0.5, op0=ALU.is_ge, op1=ALU.subtract), r=[bb], w=[bb])
                    if k + 1 < NBIS:
                        V(lambda e, k=k: e.scalar_tensor_tensor(out=MID, in0=FS, scalar=stp[:, par, k:k + 1], in1=MID, op0=ALU.mult, op1=ALU.add), r=[bb, B("stp", par)], w=[bb])
                    yield
                V(lambda e: e.tensor_scalar(out=FS, in0=FS, scalar1=-0.5, scalar2=None, op0=ALU.add), r=[bb], w=[bb])
                V(lambda e: e.scalar_tensor_tensor(out=LO, in0=FS, scalar=SL, in1=MID, op0=ALU.mult, op1=ALU.add), r=[bb, B("stp", par)], w=[bb])
            else:
                NMID = BIS[:, par * 16 + 9:par * 16 + 10]
                SSUM = BIS[:, par * 16 + 10:par * 16 + 11]
                SG = BIS[:, par * 16 + 11:par * 16 + 12]
                ba = B("bisA", par)
                V(lambda e: e.tensor_scalar(out=nstp[:, par, :], in0=stp[:, par, :], scalar1=-1.0, scalar2=None, op0=ALU.mult), r=[B("stp", par)], w=[B("nstp", par)])
                V(lambda e: e.scalar_tensor_tensor(out=NMID, in0=LO, scalar=-1.0, in1=nstp[:, par, 0:1], op0=ALU.mult, op1=ALU.add), r=[bb, B("nstp", par)], w=[ba])
                for k in range(NBIS):
                    A(lambda e: e.activation(out=junkA[:, 0:Lk], in_=sc[:, 0:Lk], func=AF.Sign, bias=NMID, scale=1.0, accum_out=SSUM), r=[bs, ba], w=[B("junkA"), ba])
                    A(lambda e: e.activation(out=SG, in_=SSUM, func=AF.Sign, bias=float(Lk - 512) + 0.5), r=[ba], w=[ba])
                    if k + 1 < NBIS:
                        A(lambda e, k=k: e.activation(out=NMID, in_=SG, func=AF.Identity, scale=nstp[:, par, k + 1:k + 2], bias=NMID), r=[ba, B("nstp", par)], w=[ba])
                    yield
                V(lambda e: e.tensor_scalar(out=FS, in0=SG, scalar1=0.5, scalar2=-0.5, op0=ALU.mult, op1=ALU.add), r=[ba], w=[bb])
                V(lambda e: e.scalar_tensor_tensor(out=LO, in0=FS, scalar=SL, in1=NMID, op0=ALU.mult, op1=ALU.subtract), r=[bb, ba, B("stp", par)], w=[bb])
        yield

    def bandsel(i):
        par, Lk, sc, bs, LO, W0, MID, CNT, FS, AM, bb = _hdr(i)
        if i < 2:
            V(lambda e: e.tensor_scalar(out=mb[i % 2][:, 0:Lk], in0=sc[:, 0:Lk], scalar1=LO, scalar2=NEG, op0=ALU.is_lt, op1=ALU.mult), r=[bs, bb], w=[B("mb", i % 2)])
        else:
            HI = BIS[:, par * 16 + 6:par * 16 + 7]
            CHI = BIS[:, par * 16 + 7:par * 16 + 8]
            NKEEP = BIS[:, par * 16 + 8:par * 16 + 9]
            V(lambda e: e.tensor_tensor(out=HI, in0=LO, in1=stp[:, par, NBIS - 1:NBIS], op=ALU.add), r=[bb, B("stp", par)], w=[bb])
            V(lambda e: e.tensor_scalar(out=gehi[:, 0:Lk], in0=sc[:, 0:Lk], scalar1=HI, scalar2=None, op0=ALU.is_ge, op1=ALU.add, accum_out=CHI), r=[bs, bb], w=[B("gehi"), bb])
            yield
            V(lambda e: e.tensor_scalar(out=NKEEP, in0=CHI, scalar1=-1.0, scalar2=256.0, op0=ALU.mult, op1=ALU.add), r=[bb], w=[bb])
            V(lambda e: e.scalar_tensor_tensor(out=bandb[:, 0:Lk], in0=sc[:, 0:Lk], scalar=LO, in1=gehi[:, 0:Lk], op0=ALU.is_ge, op1=ALU.subtract), r=[bs, bb, B("gehi")], w=[B("bandb")])
            yield
            V(lambda e: e.tensor_tensor_scan(out=cum[:, 0:Lk], data0=onesb[:, 0:Lk], data1=bandb[:, 0:Lk], initial=0.0, op0=ALU.mult, op1=ALU.add), r=[B("bandb"), B("onesb")], w=[B("cum")])
            yield
            V(lambda e: e.scalar_tensor_tensor(out=bandb[:, 0:Lk], in0=cum[:, 0:Lk], scalar=NKEEP, in1=bandb[:, 0:Lk], op0=ALU.is_le, op1=ALU.mult), r=[B("cum"), bb, B("bandb")], w=[B("bandb")])
            yield
            P(lambda e: e.tensor_scalar(out=gehi[:, 0:Lk], in0=gehi[:, 0:Lk], scalar1=-NEG, scalar2=NEG, op0=ALU.mult, op1=ALU.add), r=[B("gehi"), B("bandb")], w=[B("gehi")])
            V(lambda e: e.scalar_tensor_tensor(out=mb[i % 2][:, 0:Lk], in0=bandb[:, 0:Lk], scalar=-NEG, in1=gehi[:, 0:Lk], op0=ALU.mult, op1=ALU.add), r=[B("bandb"), B("gehi")], w=[B("mb", i % 2)])

    unit_ctr = [0]

    def attention(i):
        par = i % 2
        nk = i + 1
        units = []
        for h in range(8):
            for u0 in range(0, nk, 4):
                units.append((h, u0, min(nk, u0 + 4)))
        pend = None
        rd_q = [B("qT", i), B("qTa_aug"), B("kTa_aug")] + [B("kT", j) for j in range(nk)]

        def emit_qk(h, u0, u1, uidx):
            psu = bank(2 + uidx % 3)
            bps = B("bk", 2 + uidx % 3)
            for j in range(u0, u1):
                cs = (j - u0) * 128
                T(lambda e, j=j, cs=cs: e.matmul(psu[:, cs:cs + 128], lhsT=kTa[0:67, j * 128:(j + 1) * 128], rhs=qTa[0:67, h, i * 128:(i + 1) * 128], start=True, stop=False),
                  r=rd_q, w=[bps])
                T(lambda e, j=j, cs=cs: e.matmul(psu[:, cs:cs + 128], lhsT=mb[par][:, j * 128:(j + 1) * 128], rhs=identb[:], start=False, stop=(j != i)),
                  r=[B("mb", par), B("identb")], w=[bps])
                if j == i:
                    T(lambda e, cs=cs: e.matmul(psu[:, cs:cs + 128], lhsT=identb[:], rhs=dmat[:, h, :], start=False, stop=True),
                      r=[B("dmat"), B("identb")], w=[bps])
            slot = uidx % 3
            ncol = (u1 - u0) * 128
            A(lambda e: e.activation(out=PT[slot][:, 0:ncol], in_=psu[:, 0:ncol], func=AF.Exp), r=[bps], w=[B("PT", slot)])

        def emit_pv(h, u0, u1, uidx):
            slot = uidx % 3
            pv = bank(6 + h // 4)
            hc = (h % 4) * 65
            for j in range(u0, u1):
                cs = (j - u0) * 128
                T(lambda e, j=j, cs=cs: e.matmul(pv[:, hc:hc + 65], lhsT=PT[slot][:, cs:cs + 128], rhs=v_aug[:, j, 0:65], start=(j == 0), stop=(j == i)),
                  r=[B("PT", slot), B("v", j), B("v_ones")], w=[B("bk", 6 + h // 4)])

        for (h, u0, u1) in units:
            uidx = unit_ctr[0]
            unit_ctr[0] += 1
            emit_qk(h, u0, u1, uidx)
            if pend is not None:
                emit_pv(*pend)
            pend = (h, u0, u1, uidx)
            yield
        emit_pv(*pend)
        yield
        for g in range(2):
            pv3 = bank(6 + g)[:, 0:260].rearrange("p (a b) -> p a b", b=65)
            V(lambda e, g=g, pv3=pv3: e.tensor_scalar(out=rs[:, g * 4:(g + 1) * 4], in0=pv3[:, :, 64], scalar1=1e-30, scalar2=None, op0=ALU.add), r=[B("bk", 6 + g)], w=[B("rs")])
        V(lambda e: e.reciprocal(out=rs[:], in_=rs[:]), r=[B("rs")], w=[B("rs")])
        for g in range(2):
            pv3 = bank(6 + g)[:, 0:260].rearrange("p (a b) -> p a b", b=65)
            V(lambda e, g=g, pv3=pv3: e.tensor_tensor(out=attn_tm[:, g * 256:(g + 1) * 256].rearrange("p (a b) -> p a b", b=64), in0=pv3[:, :, 0:64],
                                                   in1=rs[:, g * 4:(g + 1) * 4, None].to_broadcast([128, 4, 64]), op=ALU.mult),
              r=[B("bk", 6 + g), B("rs")], w=[B("attn_tm")])
        tp = bankbf(5).rearrange("p (a b) -> p a b", b=128)
        for cc in range(4):
            T(lambda e, cc=cc: e.transpose(tp[:, cc, :], attn_tm[:, cc * 128:(cc + 1) * 128], identb[:]), r=[B("attn_tm"), B("identb")], w=[B("bk", 5)])
        A(lambda e: e.activation(out=attnT[:, :, i * 128:(i + 1) * 128], in_=tp[:, 0:4, :], func=AF.Copy), r=[B("bk", 5)], w=[B("attnT", i)])

    g_ = lambda f, k: f(k) if 0 <= k < NT else None
    interleave(idx_scores(0))
    interleave(bisect(0), idx_scores(1))
    interleave(bandsel(0), bisect(1), idx_scores(2))
    for i in range(NT):
        interleave(attention(i), g_(bandsel, i + 1), g_(bisect, i + 2), g_(idx_scores, i + 3))

    S.barrier()
    if stop == "B":
        finish([("attnT", attnT[:], [128, 4, SEQ], BF16), ("mb1", mb[1][:], [128, SEQ], BF16),
                ("score1", score[0][:], [128, SEQ], F32)])
        return

    M2.reset()
    L.reset()
    mergedT = M2.alloc("mergedT", [128, 8, SEQ], BF16)
    Wout = M2.alloc("Wout", [128, 8, D], BF16)
    wg = [M2.alloc(f"wg{i}", [128, 8, 256], BF16) for i in range(2)]
    h2T = L.alloc("h2T", [128, 8, SEQ], BF16)
    comb = L.alloc("comb", [128, NT, 32], F32)
    Lkeep = L.off
    wrt = L.alloc("wrt", [128, 8, 36], F32)
    logits = L.alloc("logits", [128, NT, 36], F32)
    Lov = L.off
    wp = [L.alloc(f"wp{i}", [128, 4, 256], BF16) for i in range(2)]
    wst = [L.alloc(f"cwst{i}", [128, 8, 128], F32) for i in range(2)]
    sa = L.alloc("sa", [128, 512], F32)
    sb_ = L.alloc("sb_", [128, 512], F32)
    m1 = L.alloc("m1", [128, 512], F32)
    m2 = L.alloc("m2", [128, 512], F32)
    L.off = Lov
    xin = [L.alloc(f"cxin{i}", [128, D], F32) for i in range(3)]
    sqj2 = L.alloc("sqj2", [128, D], BF16)
    h2fs = [L.alloc(f"h2f{i}", [128, 8, 128], F32) for i in range(3)]
    lgTs = [L.alloc(f"lgT{i}", [128, 128], F32) for i in range(2)]
    x1 = nc.alloc_sbuf_tensor_at("x1", [128, NT, D], F32, offset=BASE + 13 * 1024)

    wpa_v = wpa_d.rearrange("(kc p) n -> p kc n", p=128)
    wpb_v = wpb_d.rearrange("(kc p) n -> p kc n", p=128)
    wout_v = wout_d.rearrange("(kc p) n -> p kc n", p=128)
    S.dma("pool", wrt[:], wrt_d.rearrange("(kc p) n -> p kc n", p=128), writes=[B("wrt")])

    def c1_loads(n):
        ws = n % 2
        bwg, bwp = B("wg", ws), B("wp", ws)
        load_cast(wg[ws][:, :, 0:128], bwg, 1988 + 128 * n, 128)
        load_cast(wg[ws][:, :, 128:256], bwg, 3012 + 128 * n, 128)
        load_cast(wp[ws][:, :, 0:128], bwp, 128 * n, 128, dram_v=wpa_v)
        load_cast(wp[ws][:, :, 128:256], bwp, 128 * n, 128, dram_v=wpb_v)

    c1_loads(0)
    for n in range(8):
        ws = n % 2
        bwg, bwp = B("wg", ws), B("wp", ws)
        if n + 1 < 8:
            c1_loads(n + 1)
        if n == 1:
            for q4 in range(8):
                slot = wst_n[0] % 2
                wst_n[0] += 1
                st = wst[slot]
                bst = B("wst", slot)
                S.dma("sp", st[:], wout_v[:, :, q4 * 128:(q4 + 1) * 128], writes=[bst])
                P(lambda e, st=st, q4=q4: e.tensor_tensor(out=Wout[:, :, q4 * 128:(q4 + 1) * 128], in0=st[:], in1=gate_m[:, q4 * 128:(q4 + 1) * 128].rearrange("p (o n) -> p o n", o=1).to_broadcast([128, 8, 128]), op=ALU.mult),
                  r=[bst, B("gate", 2)], w=[B("Wout")])
        for tb in range(4):
            base = 4 * ((n * 4 + tb) % 2)
            kya, kyb, kga, kgb = base, base + 1, base + 2, base + 3
            cs = slice(tb * 512, (tb + 1) * 512)
            rdh = [B("hT", 4 * tb + j) for j in range(4)]
            for kc in range(4):
                T(lambda e, kc=kc: e.matmul(bank(kya), lhsT=wp[ws][:, kc, 0:128], rhs=attnT[:, kc, cs], start=(kc == 0), stop=(kc == 3)),
                  r=[bwp] + [B("attnT", 4 * tb + j) for j in range(4)], w=[B("bk", kya)])
            for kc in range(4):
                T(lambda e, kc=kc: e.matmul(bank(kyb), lhsT=wp[ws][:, kc, 128:256], rhs=hgT[:, kc, cs], start=(kc == 0), stop=(kc == 3)),
                  r=[bwp, B("hgT", tb)], w=[B("bk", kyb)])
            for kc in range(8):
                T(lambda e, kc=kc: e.matmul(bank(kga), lhsT=wg[ws][:, kc, 0:128], rhs=hT[:, kc, cs], start=(kc == 0), stop=(kc == 7)), r=[bwg] + rdh, w=[B("bk", kga)])
            for kc in range(8):
                T(lambda e, kc=kc: e.matmul(bank(kgb), lhsT=wg[ws][:, kc, 128:256], rhs=hT[:, kc, cs], start=(kc == 0), stop=(kc == 7)), r=[bwg] + rdh, w=[B("bk", kgb)])
            A(lambda e: e.activation(out=sa[:], in_=bank(kga), func=AF.Tanh, scale=0.5), r=[B("bk", kga)], w=[B("sa")])
            A(lambda e: e.activation(out=sb_[:], in_=bank(kgb), func=AF.Tanh, scale=0.5), r=[B("bk", kgb)], w=[B("sb")])
            V(lambda e: e.scalar_tensor_tensor(out=m1[:], in0=sa[:], scalar=1.0, in1=bank(kya), op0=ALU.add, op1=ALU.mult), r=[B("sa"), B("bk", kya)], w=[B("m1")])
            V(lambda e: e.scalar_tensor_tensor(out=m2[:], in0=sb_[:], scalar=1.0, in1=bank(kyb), op0=ALU.add, op1=ALU.mult), r=[B("sb"), B("bk", kyb)], w=[B("m2")])
            V(lambda e: e.tensor_tensor(out=m1[:], in0=m1[:], in1=m2[:], op=ALU.add), r=[B("m1"), B("m2")], w=[B("m1")])
            A(lambda e, n=n, cs=cs: e.activation(out=mergedT[:, n, cs], in_=m1[:], func=AF.Identity, scale=0.5), r=[B("m1")], w=[B("mergedT", tb)])

    S.barrier()
    def c2_tile(i):
        sl = i % 2
        s3 = i % 3
        bx = B("cxin", s3)
        xn2 = xin[s3]
        h2f = h2fs[s3]
        bxn, bhf = bx, B("h2f", s3)
        S.dma("pool", xin[s3][:], x_d[i * 128:(i + 1) * 128, :], writes=[bx])
        pso = PS[0]
        bpo = B("PSU", 0)
        for hf in range(2):
            for kc in range(8):
                T(lambda e, kc=kc, hf=hf: e.matmul(pso[:, hf * 512:(hf + 1) * 512], lhsT=mergedT[:, kc, i * 128:(i + 1) * 128], rhs=Wout[:, kc, hf * 512:(hf + 1) * 512], start=(kc == 0), stop=(kc == 7)),
                  r=[B("mergedT", i // 4), B("Wout")], w=[bpo])
        yield
        bx1 = B("x1", i)
        V(lambda e: e.tensor_tensor(out=x1[:, i, :], in0=pso[:, :], in1=xin[s3][:], op=ALU.add), r=[bpo, bx], w=[bx1])
        yield
        A(lambda e: e.activation(out=sqj2[:], in_=x1[:, i, :], func=AF.Square, accum_out=SS2[:, i:i + 1]), r=[bx1], w=[B("sqj2"), B("ss2", i)])
        yield
        V(lambda e: e.tensor_scalar(out=VAR2[:, i:i + 1], in0=SS2[:, i:i + 1], scalar1=1.0 / D, scalar2=EPS, op0=ALU.mult, op1=ALU.add), r=[B("ss2", i)], w=[B("var2", i)])
        P(lambda e: e.tensor_tensor(out=RSTD2[:, i:i + 1], in0=VAR2[:, i:i + 1], in1=NHALF, op=ALU.pow), r=[B("var2", i), B("nhalf")], w=[B("rstd2", i)])
        yield
        V(lambda e: e.tensor_scalar(out=xn2[:], in0=x1[:, i, :], scalar1=RSTD2[:, i:i + 1], scalar2=None, op0=ALU.mult), r=[bx1, B("rstd2", i)], w=[bxn])
        yield
        pst = PS[2 + sl]
        kb0, kb1 = 4 + 2 * sl, 5 + 2 * sl
        for dc in range(8):
            T(lambda e, dc=dc: e.transpose(pst[:, dc * 128:(dc + 1) * 128], xn2[:, dc * 128:(dc + 1) * 128], identf[:]), r=[bxn, B("identf")], w=[B("bk", kb0 + dc // 4)])
        yield
        for dc in range(8):
            if dc < 4:
                A(lambda e, dc=dc: e.activation(out=h2f[:, dc, :], in_=pst[:, dc * 128:(dc + 1) * 128], func=AF.Identity, scale=A_F[:, dc:dc + 1], bias=B_F[:, dc:dc + 1]),
                  r=[B("bk", kb0), B("AB", 1), c_sm], w=[bhf])
            else:
                V(lambda e, dc=dc: e.tensor_scalar(out=h2f[:, dc, :], in0=pst[:, dc * 128:(dc + 1) * 128], scalar1=A_F[:, dc:dc + 1], scalar2=B_F[:, dc:dc + 1], op0=ALU.mult, op1=ALU.add),
                  r=[B("bk", kb1), B("AB", 1), c_sm], w=[bhf])
        yield
        V(lambda e: e.tensor_copy(out=h2T[:, :, i * 128:(i + 1) * 128], in_=h2f[:]), r=[bhf], w=[B("h2T", i)])
        plT = bank(2 + sl)[0:36, 0:128]
        pl = bank(2 + sl)[:, 128:164]
        bkr = B("bk", 2 + sl)
        for dc in range(8):
            T(lambda e, dc=dc: e.matmul(plT, lhsT=wrt[:, dc, :], rhs=h2f[:, dc, :], start=(dc == 0), stop=(dc == 7)), r=[bhf, B("wrt")], w=[bkr])
        yield
        lgT = lgTs[sl]
        A(lambda e: e.activation(out=lgT[0:36, :], in_=plT, func=AF.Copy), r=[bkr], w=[B("lgT", sl)])
        yield
        T(lambda e: e.transpose(pl, lgT[0:36, :], identf[0:36, 0:36]), r=[B("lgT", sl), B("identf")], w=[bkr])
        yield
        V(lambda e: e.tensor_tensor(out=logits[:, i, :], in0=pl, in1=rows[:, 0:36], op=ALU.add), r=[bkr, B("rows")], w=[B("logits")])
        yield

    staggered(c2_tile, NT, depth=3)

    S.barrier()
    L.off = Lov
    r8 = L.alloc("r8", [128, NT, 8], F32)
    r8b = L.alloc("r8b", [128, NT, 8], F32)
    r32 = L.alloc("r32", [128, NT, 32], F32)
    r4 = L.alloc("r4", [128, NT, 4], F32)
    goh = L.alloc("goh", [128, NT, 4], F32)
    rv = L.alloc("rv", [128, 8, NT], F32)
    GMAX, GSUM, M1_, M2_, W1_, W2_, GW = (rv[:, k, :] for k in range(7))
    bl_, brt = B("logits"), B("route")
    gl = logits[:, :, 0:4]
    el = logits[:, :, 4:36]
    V(lambda e: e.tensor_reduce(out=GMAX, in_=gl, axis=AX.X, op=ALU.max), r=[bl_], w=[brt])
    V(lambda e: e.tensor_tensor(out=goh[:], in0=gl, in1=GMAX[:, :, None].to_broadcast([128, NT, 4]), op=ALU.is_equal), r=[bl_, brt], w=[brt])
    V(lambda e: e.tensor_tensor(out=r4[:], in0=gl, in1=GMAX[:, :, None].to_broadcast([128, NT, 4]), op=ALU.subtract), r=[bl_, brt], w=[brt])
    A(lambda e: e.activation(out=r4[:], in_=r4[:], func=AF.Exp), r=[brt], w=[brt])
    V(lambda e: e.tensor_reduce(out=GSUM, in_=r4[:], axis=AX.X, op=ALU.add), r=[brt], w=[brt])
    V(lambda e: e.reciprocal(out=GW, in_=GSUM), r=[brt], w=[brt])
    V(lambda e: e.tensor_tensor(out=r32[:].rearrange("p t (g j) -> p t g j", j=8), in0=el.rearrange("p t (g j) -> p t g j", j=8),
                                in1=goh[:, :, :, None].to_broadcast([128, NT, 4, 8]), op=ALU.mult), r=[bl_, brt], w=[brt])
    V(lambda e: e.tensor_reduce(out=r8[:], in_=r32[:].rearrange("p t (g j) -> p t j g", j=8), axis=AX.X, op=ALU.add), r=[brt], w=[brt])
    V(lambda e: e.tensor_reduce(out=M1_, in_=r8[:], axis=AX.X, op=ALU.max), r=[brt], w=[brt])
    oh1 = r8b
    V(lambda e: e.tensor_tensor(out=oh1[:], in0=r8[:], in1=M1_[:, :, None].to_broadcast([128, NT, 8]), op=ALU.is_equal), r=[brt], w=[brt])
    e2 = L.alloc("e2", [128, NT, 8], F32)
    V(lambda e: e.scalar_tensor_tensor(out=e2[:], in0=oh1[:], scalar=SENT, in1=r8[:], op0=ALU.mult, op1=ALU.add), r=[brt], w=[brt])
    V(lambda e: e.tensor_reduce(out=M2_, in_=e2[:], axis=AX.X, op=ALU.max), r=[brt], w=[brt])
    oh2 = L.alloc("oh2", [128, NT, 8], F32)
    V(lambda e: e.tensor_tensor(out=oh2[:], in0=e2[:], in1=M2_[:, :, None].to_broadcast([128, NT, 8]), op=ALU.is_equal), r=[brt], w=[brt])
    V(lambda e: e.tensor_tensor(out=W2_, in0=M2_, in1=M1_, op=ALU.subtract), r=[brt], w=[brt])
    A(lambda e: e.activation(out=W2_, in_=W2_, func=AF.Exp), r=[brt], w=[brt])
    V(lambda e: e.tensor_scalar(out=W1_, in0=W2_, scalar1=1.0, scalar2=None, op0=ALU.add), r=[brt], w=[brt])
    V(lambda e: e.reciprocal(out=W1_, in_=W1_), r=[brt], w=[brt])
    V(lambda e: e.tensor_tensor(out=W2_, in0=W2_, in1=W1_, op=ALU.mult), r=[brt], w=[brt])
    V(lambda e: e.tensor_tensor(out=W1_, in0=W1_, in1=GW, op=ALU.mult), r=[brt], w=[brt])
    V(lambda e: e.tensor_tensor(out=W2_, in0=W2_, in1=GW, op=ALU.mult), r=[brt], w=[brt])
    V(lambda e: e.tensor_tensor(out=oh1[:], in0=oh1[:], in1=W1_[:, :, None].to_broadcast([128, NT, 8]), op=ALU.mult), r=[brt], w=[brt])
    V(lambda e: e.tensor_tensor(out=oh2[:], in0=oh2[:], in1=W2_[:, :, None].to_broadcast([128, NT, 8]), op=ALU.mult), r=[brt], w=[brt])
    V(lambda e: e.tensor_tensor(out=oh1[:], in0=oh1[:], in1=oh2[:], op=ALU.add), r=[brt], w=[brt])
    for g in range(4):
        V(lambda e, g=g: e.tensor_tensor(out=comb[:, :, g * 8:(g + 1) * 8], in0=oh1[:], in1=goh[:, :, g:g + 1].to_broadcast([128, NT, 8]), op=ALU.mult), r=[brt], w=[B("comb")])

    S.barrier()
    if stop == "C":
        finish([("x1", x1[:], [128, NT, D], F32), ("h2T", h2T[:], [128, 8, SEQ], BF16), ("comb", comb[:], [128, NT, 32], F32),
                ("logits", logits[:], [128, NT, 36], F32)])
        return

    M2.reset()
    L.off = Lkeep
    W13 = [M2.alloc(f"W13_{i}", [128, 8, 512], BF16) for i in range(2)]
    W2b = [M2.alloc(f"W2b_{i}", [128, 2, D], BF16) for i in range(2)]
    mst = [M2.alloc(f"mst{i}", [128, 2048], F32) for i in range(3)]
    s_t2 = [L.alloc(f"s_t{i}", [128, 512], F32) for i in range(2)]
    actT2 = [L.alloc(f"actT{i}", [128, 2, 512], BF16) for i in range(2)]
    mst_n = [0]

    def moe_load(e_):
        ws = e_ % 2
        for which, (src, dst3, isw2) in enumerate([
            (w1_d[e_].rearrange("(kc p) f -> p kc f", p=128), W13[ws][:, :, 0:256], False),
            (w3_d[e_].rearrange("(kc p) f -> p kc f", p=128), W13[ws][:, :, 256:512], False),
            (w2_d[e_].rearrange("(fc p) n -> p fc n", p=128), W2b[ws][:, :, :], True)]):
            slot = mst_n[0] % 3
            mst_n[0] += 1
            bst = B("mst", slot)
            if isw2:
                stv = mst[slot][:, :].rearrange("p (a b) -> p a b", b=D)
                S.dma("sp", stv, src, writes=[bst])
                P(lambda e, stv=stv, dst3=dst3: e.tensor_tensor(out=dst3, in0=stv, in1=gate_f[:, :].rearrange("p (o n) -> p o n", o=1).to_broadcast([128, 2, D]), op=ALU.mult),
                  r=[bst, B("gate", 5)], w=[B("W2b", ws)])
            else:
                stv = mst[slot][:, :].rearrange("p (a b) -> p a b", b=256)
                S.dma("sp", stv, src, writes=[bst])
                P(lambda e, stv=stv, dst3=dst3: e.tensor_copy(out=dst3, in_=stv), r=[bst], w=[B("W13", ws)])

    units = [(e_, g) for e_ in range(NEXP) for g in range(4)]
    NU = len(units)
    out_toks = []

    def u_up(q, fcs=(0, 1)):
        e_, g = units[q]
        ws = e_ % 2
        aT = actT2[q % 2]
        baT = B("actT2", q % 2)
        cs = slice(g * 512, (g + 1) * 512)
        rdh = [B("h2T", 4 * g + j) for j in range(4)]
        for fc in fcs:
            hsel = (2 * q + fc) % 2
            k1, k3 = 2 * hsel, 2 * hsel + 1
            for dc in range(8):
                T(lambda e, dc=dc: e.matmul(bank(k1), lhsT=W13[ws][:, dc, fc * 128:(fc + 1) * 128], rhs=h2T[:, dc, cs], start=(dc == 0), stop=(dc == 7)),
                  r=rdh + [B("W13", ws)], w=[B("bk", k1)])
            for dc in range(8):
                T(lambda e, dc=dc: e.matmul(bank(k3), lhsT=W13[ws][:, dc, 256 + fc * 128:256 + (fc + 1) * 128], rhs=h2T[:, dc, cs], start=(dc == 0), stop=(dc == 7)),
                  r=rdh + [B("W13", ws)], w=[B("bk", k3)])
            st_ = s_t2[hsel]
            A(lambda e: e.activation(out=st_[:], in_=bank(k1), func=AF.Silu), r=[B("bk", k1)], w=[B("s_t2", hsel)])
            V(lambda e: e.tensor_tensor(out=aT[:, fc, :], in0=bank(k3), in1=st_[:], op=ALU.mult), r=[B("bk", k3), B("s_t2", hsel)], w=[baT])

    dn_ctr = [0]

    def u_down(q, js=(0, 1, 2, 3)):
        e_, g = units[q]
        ws = e_ % 2
        aT = actT2[q % 2]
        baT = B("actT2", q % 2)
        for j in js:
            i = 4 * g + j
            d_ = dn_ctr[0] % 2
            dn_ctr[0] += 1
            pd = PS[2 + d_]
            bpd = B("PSU", 2 + d_)
            for hf in range(2):
                for fc in range(2):
                    T(lambda e, hf=hf, fc=fc: e.matmul(pd[:, hf * 512:(hf + 1) * 512], lhsT=aT[:, fc, j * 128:(j + 1) * 128], rhs=W2b[ws][:, fc, hf * 512:(hf + 1) * 512], start=(fc == 0), stop=(fc == 1)),
                      r=[baT, B("W2b", ws)], w=[bpd])
            bx1 = B("x1", i)
            V(lambda e, i=i: e.scalar_tensor_tensor(out=x1[:, i, :], in0=pd[:, :], scalar=comb[:, i, e_:e_ + 1], in1=x1[:, i, :], op0=ALU.mult, op1=ALU.add),
              r=[bpd, B("comb"), bx1], w=[bx1])
            if e_ == NEXP - 1:
                out_toks.append(S.dma("pool", out_d[i * 128:(i + 1) * 128, :], x1[:, i, :], reads=[bx1]))

    moe_load(0)
    for q in range(NU + 1):
        if q < NU:
            e_, g = units[q]
            if g == 1 and e_ + 1 < NEXP:
                moe_load(e_ + 1)
            u_up(q, (0,))
            if q >= 1:
                u_down(q - 1, (0, 1))
            u_up(q, (1,))
            if q >= 1:
                u_down(q - 1, (2, 3))
        else:
            u_down(q - 1)

    for t in out_toks:
        S._wait("pool", t)
    S.barrier()


_CACHE = {}


def _consts():
    s = np.arange(SEQ)
    ktab = np.stack([128.0 * (s // 128), (s % 128).astype(np.float64), np.ones(SEQ)]).astype(np.float32)
    slopes = 2.0 ** (-(np.arange(1, 9)).astype(np.float64))
    qtab = np.zeros((3, 8, SEQ), np.float32)
    for h in range(8):
        qtab[0, h] = slopes[h]
        qtab[1, h] = slopes[h]
        qtab[2, h] = -slopes[h] * (128.0 * (s // 128) + 64.0)
    sl = np.arange(128)
    rel = np.maximum(sl[:, None] - sl[None, :], 0).astype(np.float64)
    dmat = np.zeros((128, 8, 128), np.float32)
    for h in range(8):
        dmat[:, h, :] = -2.0 * slopes[h] * rel
    bf = ml_dtypes.bfloat16
    return dict(ktab=ktab.astype(bf), qtab=qtab.astype(bf), dmat=dmat.astype(bf),
                identb=np.eye(128, dtype=np.float32).astype(bf), identf=np.eye(128, dtype=np.float32))


def kernel(x, c, ada_w, ada_b, norm_mix_w, w_in, q_norm_w, k_norm_w, conv_w, conv_b,
           w_rec_gate, b_rec_gate, w_in_gate, b_in_gate, lru_lambda, w_proj_a, w_proj_b,
           w_out, norm_ffn_w, w_group, b_group, w_expert_router, b_expert_router, w1, w3, w2):
    f = lambda a: np.ascontiguousarray(np.asarray(a, dtype=np.float32))
    col = lambda v: f(v).reshape(-1, 128).T
    x = f(x); c = f(c)
    ab = f(ada_b)[0]
    cw = f(conv_w)[0]
    shared = [col(norm_mix_w[0]), col(norm_ffn_w[0]), col(ab[0:D]), col(ab[D:2 * D]), col(ab[3 * D:4 * D]), col(ab[4 * D:5 * D])]
    shared += [col(cw[j]) for j in range(4)]
    shared += [col(f(conv_b)[0]), col(f(b_rec_gate)[0]), col(f(b_in_gate)[0]), col(f(lru_lambda)[0])]
    shared = np.concatenate(shared, axis=1)
    rows = np.concatenate([f(b_group)[0], f(b_expert_router)[0]])[None, :].repeat(128, axis=0)
    gbias = np.concatenate([ab[2 * D:3 * D], ab[5 * D:6 * D]])[None, :].repeat(128, axis=0)
    qkw = np.concatenate([np.tile(f(q_norm_w)[0], 8), f(k_norm_w)[0]])[None, :].repeat(128, axis=0)
    wbd = np.zeros((128, 2, 4, 128), np.float32)
    for g, wsrc in enumerate((f(w_rec_gate)[0], f(w_in_gate)[0])):
        for cc in range(4):
            for bl in range(2):
                wbd[bl * 64:(bl + 1) * 64, g, cc, bl * 64:(bl + 1) * 64] = wsrc[2 * cc + bl]
    w_rt = np.concatenate([f(w_group)[0], f(w_expert_router)[0]], axis=1)
    common = dict(rows=f(rows), gbias=f(gbias), qkw=f(qkw), ada_w=f(ada_w)[0], w_in=f(w_in)[0], w_proj_a=f(w_proj_a)[0], w_proj_b=f(w_proj_b)[0],
                  w_out=f(w_out)[0], w_rt=f(w_rt), wbd=wbd, w1=f(w1)[0], w3=f(w3)[0], w2=f(w2)[0])
    common.update(_consts())
    in_maps = []
    for b in range(8):
        m = dict(common)
        m["x"] = x[b]
        m["cols"] = np.ascontiguousarray(np.concatenate([col(c[b]), shared], axis=1))
        in_maps.append(m)
    if "nc" not in _CACHE:
        _CACHE["nc"] = build_program()
    res = run_bass_kernel_spmd(_CACHE["nc"], in_maps, core_ids=list(range(8)))
    return np.stack([np.asarray(r["out"], dtype=np.float32) for r in res.results], axis=0)
```

```python
import numpy as np
from contextlib import ExitStack
import concourse.bass as bass
import concourse.mybir as mybir
from concourse.bass_utils import run_bass_kernel_spmd
import ml_dtypes

F32 = mybir.dt.float32
BF16 = mybir.dt.bfloat16
ALU = mybir.AluOpType
AF = mybir.ActivationFunctionType
AX = mybir.AxisListType

D = 1024
SEQ = 2048
NT = 16
D_IN = 4036
EPS = 1e-6
NEXP = 32
NBIS = 12
ACT_BIS_FROM = 4
NCOL = 88
NROW = 36
NEG = -32768.0
SENT = -1.0e30


class Buf:
    __slots__ = ("name", "w", "r")

    def __init__(self, name):
        self.name = name
        self.w = None
        self.r = []


class Sched:
    def __init__(self, nc, es, ndma=16):
        self.nc = nc
        self.eng = {"pe": nc.tensor, "act": nc.scalar, "dve": nc.vector, "pool": nc.gpsimd, "sp": nc.sync}
        self.sem = {k: es.enter_context(nc.semaphore("s_" + k)) for k in self.eng}
        self.cnt = {k: 0 for k in self.eng}
        self.seen = {k: {} for k in self.eng}
        self.nd = ndma
        self.dsem = [es.enter_context(nc.semaphore(f"d{i}")) for i in range(2 * ndma)]
        self.dval = [0] * (2 * ndma)
        self.dnext = {"sp": 0, "pool": 0}
        self.bufs = {}
        self.dma_toks = []

    def B(self, *key):
        b = self.bufs.get(key)
        if b is None:
            b = self.bufs[key] = Buf(key)
        return b

    def _wait(self, e, tok):
        if tok is None:
            return
        kind, idx, val = tok
        if kind == "e" and idx == e and e == "pe":
            return
        key = (kind, idx)
        if self.seen[e].get(key, 0) >= val:
            return
        sem = self.sem[idx] if kind == "e" else self.dsem[idx]
        self.eng[e].wait_ge(sem, val)
        self.seen[e][key] = val

    def _deps(self, e, reads, writes):
        for b in reads:
            self._wait(e, b.w)
            if b.name[0] in ("bk", "PSU"):
                for t in b.r:
                    if t[1] != e:
                        self._wait(e, t)
        for b in writes:
            self._wait(e, b.w)
            for t in b.r:
                self._wait(e, t)

    def _commit(self, tok, reads, writes):
        for b in writes:
            b.w = tok
            b.r = []
        for b in reads:
            b.r.append(tok)

    def op(self, e, fn, reads=(), writes=()):
        self._deps(e, reads, writes)
        ins = fn(self.eng[e])
        self.cnt[e] += 1
        ins.then_inc(self.sem[e], 1)
        tok = ("e", e, self.cnt[e])
        self._commit(tok, reads, writes)
        return tok

    def dma(self, q, out, in_, reads=(), writes=()):
        i = self.dnext[q] + (0 if q == "sp" else self.nd)
        self.dnext[q] = (self.dnext[q] + 1) % self.nd
        if self.dval[i]:
            self._wait(q, ("d", i, self.dval[i]))
        self._deps(q, reads, writes)
        self.dval[i] += 16
        self.eng[q].dma_start(out=out, in_=in_).then_inc(self.dsem[i], 16)
        tok = ("d", i, self.dval[i])
        self._commit(tok, reads, writes)
        self.dma_toks.append(tok)
        return tok

    def barrier(self):
        engs = ["pe", "act", "dve", "pool"]
        for e in engs + ["sp"]:
            for o in engs:
                if self.cnt[o] and not (o == e == "pe"):
                    self._wait(e, ("e", o, self.cnt[o]))
            for i, v in enumerate(self.dval):
                if v:
                    self._wait(e, ("d", i, v))


class Arena:
    def __init__(self, nc, base, size, tag):
        self.nc, self.base, self.size, self.tag = nc, base, size, tag
        self.off = 0
        self.n = 0

    def alloc(self, name, shape, dt):
        nb = int(np.prod(shape[1:])) * (2 if dt == BF16 else 4)
        nb = (nb + 63) // 64 * 64
        assert self.off + nb <= self.size, (self.tag, name, self.off, nb, self.size)
        self.n += 1
        t = self.nc.alloc_sbuf_tensor_at(f"{self.tag}_{name}_{self.n}", list(shape), dt, offset=self.base + self.off)
        self.off += nb
        return t

    def reset(self):
        self.off = 0


def build_program(stop=None):
    nc = bass.Bass("TRN2", target_bir_lowering=False)
    with ExitStack() as es:
        _body(nc, es, stop)
    return nc


def _body(nc, es, stop):

    def din(name, shape, dt=F32):
        return nc.dram_tensor(name, list(shape), dt, kind="ExternalInput").ap()

    x_d = din("x", [SEQ, D])
    cols_d = din("cols", [128, NCOL])
    rows_d = din("rows", [128, NROW])
    gbias_d = din("gbias", [128, 2 * D])
    qkw_d = din("qkw", [128, 576])
    adaw_d = din("ada_w", [D, 6 * D])
    win_d = din("w_in", [D, D_IN])
    wpa_d = din("w_proj_a", [512, D])
    wpb_d = din("w_proj_b", [512, D])
    wout_d = din("w_out", [D, D])
    wrt_d = din("w_rt", [D, 36])
    wbd_d = din("wbd", [128, 2, 4, 128])
    w1_d = din("w1", [NEXP, D, 256])
    w3_d = din("w3", [NEXP, D, 256])
    w2_d = din("w2", [NEXP, 256, D])
    ktab_d = din("ktab", [3, SEQ], BF16)
    qtab_d = din("qtab", [3, 8, SEQ], BF16)
    dmat_d = din("dmat", [128, 8, 128], BF16)
    identb_d = din("identb", [128, 128], BF16)
    identf_d = din("identf", [128, 128])
    out_d = nc.dram_tensor("out", [SEQ, D], F32, kind="ExternalOutput").ap()
    dbg_d = {}

    win_v = win_d.rearrange("(kc p) n -> p kc n", p=128)
    adaw_v = adaw_d.rearrange("(kc p) n -> p kc n", p=128)

    S = Sched(nc, es)
    B = S.B
    V = lambda fn, r=(), w=(): S.op("dve", fn, r, w)
    A = lambda fn, r=(), w=(): S.op("act", fn, r, w)
    P = lambda fn, r=(), w=(): S.op("pool", fn, r, w)
    T = lambda fn, r=(), w=(): S.op("pe", fn, r, w)

    def finish(dumps):
        toks = []
        S.barrier()
        for name, ap, shape, dt in dumps:
            d = nc.dram_tensor("dbg_" + name, list(shape), dt, kind="ExternalOutput").ap()
            toks.append(S.dma("pool", d, ap))
        for t in toks:
            S._wait("pool", t)
        S.barrier()

    def staggered_g(make_gen, n, depth=2):
        active = []
        nxt = 0
        while nxt < n or active:
            if nxt < n and len(active) < depth:
                active.append(make_gen(nxt))
                nxt += 1
            for g in list(active):
                try:
                    next(g)
                except StopIteration:
                    active.remove(g)
            yield

    def staggered(make_gen, n, depth=2):
        for _ in staggered_g(make_gen, n, depth):
            pass

    def interleave(*gens):
        gens = [g for g in gens if g is not None]
        while gens:
            for g in list(gens):
                try:
                    next(g)
                except StopIteration:
                    gens.remove(g)

    BASE = 16512
    TOP = 229344
    G = Arena(nc, BASE, 13 * 1024, "G")
    R64 = Arena(nc, BASE + 13 * 1024, 64 * 1024, "R")
    M2 = Arena(nc, BASE + 77 * 1024, 57 * 1024, "M")
    L = Arena(nc, BASE + 134 * 1024, TOP - (BASE + 134 * 1024), "L")

    PS = [nc.alloc_psum_tensor(f"ps{i}", [128, 1024], F32) for i in range(4)]

    def bank(k):
        return PS[k // 2][:, (k % 2) * 512:(k % 2) * 512 + 512]

    def bankbf(k):
        return PS[k // 2][:, :].bitcast(BF16)[:, (k % 2) * 1024:(k % 2) * 1024 + 1024]

    identb = G.alloc("identb", [128, 128], BF16)
    identf = G.alloc("identf", [128, 128], F32)
    cols = G.alloc("cols", [128, NCOL], F32)
    rows = G.alloc("rows", [128, NROW], F32)
    ones_r = G.alloc("ones_r", [1, 128], F32)
    gate_m = G.alloc("gate_m", [128, D], F32)
    gate_f = G.alloc("gate_f", [128, D], F32)
    sm = G.alloc("sm", [128, 256], F32)
    sm2 = G.alloc("sm2", [128, 64], F32)
    modp = G.alloc("modp", [128, 32], F32)
    dgt = G.alloc("dgt", [128, 128], F32)
    cond_bc = nc.alloc_sbuf_tensor_at("cond_bc", [128, 8, 128], F32, offset=TOP - 4096 - 64)
    COND = sm[:, 0:8]
    MODC = sm[:, 8:40]
    A_M = sm[:, 40:48]
    B_M = sm[:, 8:16]
    A_F = sm[:, 48:56]
    B_F = sm[:, 24:32]
    SS = sm[:, 56:72]
    RSTD = sm[:, 72:88]
    VAR = sm[:, 88:104]
    NHALF = sm[:, 104:105]
    NH16 = sm[:, 228:244]
    CA = sm[:, 105:109]
    SPT = sm[:, 109:113]
    SSQ = sm[:, 113:122]
    RQ = sm[:, 122:131]
    VQ = sm[:, 131:140]
    HBT = sm[:, 140:144]
    HBI = sm[:, 144:148]
    BIS = sm2[:, 0:48]
    SS2 = sm[:, 180:196]
    RSTD2 = sm[:, 196:212]
    VAR2 = sm[:, 212:228]

    c_cond = B("cond")
    c_sm = B("sm")

    S.dma("pool", cols[:], cols_d, writes=[B("cols")])
    S.dma("pool", rows[:], rows_d, writes=[B("rows")])
    S.dma("pool", gate_m[:], gbias_d[:, 0:D], writes=[B("gate", 2)])
    S.dma("pool", gate_f[:], gbias_d[:, D:2 * D], writes=[B("gate", 5)])
    S.dma("pool", identb[:], identb_d, writes=[B("identb")])
    S.dma("pool", identf[:], identf_d, writes=[B("identf")])
    V(lambda e: e.memset(sm[:], 0.0), w=[c_sm])
    V(lambda e: e.memset(sm2[:], 0.0), w=[c_sm])
    S.barrier()
    V(lambda e: e.memset(ones_r[:], 1.0), w=[B("ones_r")])
    V(lambda e: e.memset(NHALF, -0.5), w=[B("nhalf")])
    V(lambda e: e.memset(NH16, -0.5), w=[B("nhalf")])

    A(lambda e: e.activation(out=COND, in_=cols[:, 0:8], func=AF.Silu), r=[B("cols")], w=[c_cond])
    V(lambda e: e.tensor_copy(out=cond_bc[:], in_=COND[:, :, None].to_broadcast([128, 8, 128])), r=[c_cond], w=[B("cond_bc")])

    hT = R64.alloc("hT", [128, 8, SEQ], BF16)
    hgT = R64.alloc("hgT", [128, 4, SEQ], BF16)
    attnT = R64.alloc("attnT", [128, 4, SEQ], BF16)
    ada_st = [nc.alloc_sbuf_tensor_at("ada_st0", [128, 8, 512], F32, offset=BASE + 13 * 1024 + 48 * 1024),
              nc.alloc_sbuf_tensor_at("ada_st1", [128, 8, 512], F32, offset=BASE + 13 * 1024 + 32 * 1024)]

    ada_n = [0]

    ada_slot = {}

    def ada_dma(pidx, slot=None):
        if slot is None:
            slot = ada_n[0] % 2
            ada_n[0] += 1
        ada_slot[pidx] = slot
        S.dma("sp", ada_st[slot][:], adaw_v[:, :, pidx * 512:(pidx + 1) * 512], writes=[B("ada_st", slot)])

    def ada_mm(pidx, kb=None):
        slot = ada_slot[pidx]
        st = ada_st[slot]
        bst = B("ada_st", slot)
        vec = pidx // 2
        half = pidx % 2
        if vec in (2, 5):
            kg_ = 1 if kb is None else kb
            pb = bank(kg_)
            for kc in range(8):
                T(lambda e, kc=kc: e.matmul(pb, lhsT=cond_bc[:, kc, :], rhs=st[:, kc, :], start=(kc == 0), stop=(kc == 7)),
                  r=[bst, B("cond_bc")], w=[B("bk", kg_)])
            dst = gate_m if vec == 2 else gate_f
            V(lambda e: e.tensor_tensor(out=dst[:, half * 512:(half + 1) * 512], in0=pb, in1=dst[:, half * 512:(half + 1) * 512], op=ALU.add),
              r=[B("bk", kg_), B("gate", vec)], w=[B("gate", vec)])
        else:
            vi = {0: 0, 1: 1, 3: 2, 4: 3}[vec]
            kg_ = 1 if kb is None else kb
            pb = bank(kg_)
            for kc in range(8):
                T(lambda e, kc=kc: e.matmul(pb, lhsT=cond_bc[:, kc, :], rhs=st[:, kc, :], start=(kc == 0), stop=(kc == 7)),
                  r=[bst, B("cond_bc")], w=[B("bk", kg_)])
            for nn in range(4):
                col = vi * 8 + half * 4 + nn
                V(lambda e, nn=nn: e.tensor_tensor(out=dgt[:], in0=pb[:, nn * 128:(nn + 1) * 128], in1=identf[:], op=ALU.mult), r=[B("bk", kg_), B("identf")], w=[B("dgt")])
                V(lambda e, col=col: e.tensor_reduce(out=modp[:, col:col + 1], in_=dgt[:], axis=AX.X, op=ALU.add), r=[B("dgt")], w=[B("modp")])

    def ada_piece(pidx):
        ada_dma(pidx)
        ada_mm(pidx)

    def ada_finish(which):
        lo = 0 if which == 0 else 16
        cb = 24 if which == 0 else 40
        V(lambda e: e.tensor_tensor(out=MODC[:, lo:lo + 16], in0=modp[:, lo:lo + 16], in1=cols[:, cb:cb + 16], op=ALU.add),
          r=[B("modp"), B("cols")], w=[c_sm])
        nw = cols[:, 8:16] if which == 0 else cols[:, 16:24]
        dst = A_M if which == 0 else A_F
        V(lambda e: e.scalar_tensor_tensor(out=dst, in0=MODC[:, lo + 8:lo + 16], scalar=1.0, in1=nw, op0=ALU.add, op1=ALU.mult),
          r=[c_sm, B("cols")], w=[B("AB", which)])

    for p_ in range(4):
        ada_piece(p_)
    ada_finish(0)

    qTa = M2.alloc("qTa", [128, 8, SEQ], BF16)
    kTa = M2.alloc("kTa", [128, SEQ], BF16)
    v_aug = M2.alloc("v_aug", [128, NT, 66], BF16)
    qiT = M2.alloc("qiT", [128, 2, SEQ], BF16)
    kiT = M2.alloc("kiT", [128, 2, SEQ], BF16)
    widx = M2.alloc("widx", [128, NT, 4], F32)
    dmat = M2.alloc("dmat", [128, 8, 128], BF16)
    S.dma("pool", qTa[64:67, :, :], qtab_d, writes=[B("qTa_aug")])
    S.dma("pool", kTa[64:67, :], ktab_d, writes=[B("kTa_aug")])
    S.dma("pool", dmat[:], dmat_d, writes=[B("dmat")])
    P(lambda e: e.memset(v_aug[:, :, 64:66], 1.0), w=[B("v_ones")])

    wst = [L.alloc(f"wst{i}", [128, 8, 256], F32) for i in range(2)]
    wfm = [L.alloc(f"wfm{i}", [128, 8, 128], BF16) for i in range(4)]
    wbd_b = L.alloc("wbd_b", [128, 2, 4, 128], BF16)
    Lmark = L.off
    Wtm = L.alloc("Wtm", [128, 8, 644], BF16)
    wbd_f = L.alloc("wbd_f", [128, 2, 4, 128], F32)
    qkw = L.alloc("qkw", [128, 576], F32)
    xin = [L.alloc(f"xin{i}", [128, D], F32) for i in range(2)]
    xn = [L.alloc(f"xn{i}", [128, D], BF16) for i in range(2)]
    sqj = L.alloc("sqj", [128, D], BF16)
    qk2 = [L.alloc(f"qk{i}", [128, 576], F32) for i in range(2)]
    sq2 = [L.alloc(f"sq{i}", [128, 576], F32) for i in range(2)]
    qkn2 = [L.alloc(f"qkn{i}", [128, 576], BF16) for i in range(2)]

    S.dma("pool", wbd_f[:], wbd_d, writes=[B("wbd_f")])
    S.dma("pool", qkw[:], qkw_d, writes=[B("qkw")])
    P(lambda e: e.tensor_copy(out=wbd_b[:], in_=wbd_f[:]), r=[B("wbd_f")], w=[B("wbd_b")])
    P(lambda e: e.tensor_scalar(out=qkw[:, 0:512], in0=qkw[:, 0:512], scalar1=0.125, scalar2=None, op0=ALU.mult), r=[B("qkw")], w=[B("qkw")])

    wst_n = [0]

    def load_cast(dst_ap, bdst, c0, w, eng="pool", dram_v=None):
        slot = wst_n[0] % 2
        wst_n[0] += 1
        st = wst[slot]
        bst = B("wst", slot)
        src = (win_v if dram_v is None else dram_v)[:, :, c0:c0 + w]
        nk = src.shape[1]
        S.dma("sp", st[:, 0:nk, 0:w], src, writes=[bst])
        S.op(eng, lambda e: e.tensor_copy(out=dst_ap, in_=st[:, 0:nk, 0:w]), [bst], [bdst])

    bWtm = B("Wtm")
    load_cast(Wtm[:, :, 0:256], bWtm, 0, 256)
    load_cast(Wtm[:, :, 256:512], bWtm, 256, 256)
    load_cast(Wtm[:, :, 512:640], bWtm, 512, 128)
    load_cast(Wtm[:, :, 640:644], bWtm, 960, 4)

    def a1_tile(i):
        sl = i % 2
        bx = B("xin", sl)
        S.dma("pool", xin[sl][:], x_d[i * 128:(i + 1) * 128, :], writes=[bx])
        A(lambda e: e.activation(out=sqj[:], in_=xin[sl][:], func=AF.Square, accum_out=SS[:, i:i + 1]),
          r=[bx], w=[B("sqj"), B("ss", i)])
        yield
        V(lambda e: e.tensor_scalar(out=VAR[:, i:i + 1], in0=SS[:, i:i + 1], scalar1=1.0 / D, scalar2=EPS, op0=ALU.mult, op1=ALU.add),
          r=[B("ss", i)], w=[B("var", i)])
        yield
        P(lambda e: e.tensor_tensor(out=RSTD[:, i:i + 1], in0=VAR[:, i:i + 1], in1=NHALF, op=ALU.pow),
          r=[B("var", i), B("nhalf")], w=[B("rstd", i)])
        yield
        V(lambda e: e.tensor_scalar(out=xn[sl][:], in0=xin[sl][:], scalar1=RSTD[:, i:i + 1], scalar2=None, op0=ALU.mult),
          r=[bx, B("rstd", i)], w=[B("xn", sl)])
        yield
        bk = sl
        tp = bankbf(bk).rearrange("p (a b) -> p a b", b=128)
        for dc in range(8):
            T(lambda e, dc=dc: e.transpose(tp[:, dc, :], xn[sl][:, dc * 128:(dc + 1) * 128], identb[:]),
              r=[B("xn", sl), B("identb")], w=[B("bk", bk)])
        yield
        for dc in range(8):
            o = hT[:, dc, i * 128:(i + 1) * 128]
            if sl == 0:
                A(lambda e, dc=dc, o=o: e.activation(out=o, in_=tp[:, dc, :], func=AF.Identity, scale=A_M[:, dc:dc + 1], bias=B_M[:, dc:dc + 1]),
                  r=[B("bk", bk), B("AB", 0), c_sm], w=[B("hT", i)])
            else:
                V(lambda e, dc=dc, o=o: e.tensor_scalar(out=o, in0=tp[:, dc, :], scalar1=A_M[:, dc:dc + 1], scalar2=B_M[:, dc:dc + 1], op0=ALU.mult, op1=ALU.add),
                  r=[B("bk", bk), B("AB", 0), c_sm], w=[B("hT", i)])
            if dc % 4 == 3:
                yield

    if stop == "A1":
        staggered(a1_tile, NT)
    if stop == "A1":
        finish([("hT", hT[:], [128, 8, SEQ], BF16), ("sm", sm[:], [128, 256], F32)])
        return

    if stop == "A1b":
        finish([("gate_m", gate_m[:], [128, D], F32), ("gate_f", gate_f[:], [128, D], F32), ("sm", sm[:], [128, 256], F32)])
        return
    def a2_tile(i):
        st_ = i % 2
        kq, kk, ktq, ktk = (4, 5, 6, 5) if st_ == 0 else (7, 2, 3, 2)
        qk_, sq_, qkn_ = qk2[st_], sq2[st_], qkn2[st_]
        SSQ_, RQ_, VQ_ = sm2[:, st_ * 32:st_ * 32 + 9], sm2[:, st_ * 32 + 9:st_ * 32 + 18], sm2[:, st_ * 32 + 18:st_ * 32 + 27]
        bqk, bsq, bqkn, bv_ = B("qk", st_), B("sq", st_), B("qkn", st_), B("vq", st_)
        bh = B("hT", i)
        for dc in range(8):
            T(lambda e, dc=dc: e.matmul(bank(kq), lhsT=hT[:, dc, i * 128:(i + 1) * 128], rhs=Wtm[:, dc, 0:512], start=(dc == 0), stop=(dc == 7)),
              r=[bh, bWtm], w=[B("bk", kq)])
        for dc in range(8):
            T(lambda e, dc=dc: e.matmul(bank(kk)[:, 0:132], lhsT=hT[:, dc, i * 128:(i + 1) * 128], rhs=Wtm[:, dc, 512:644], start=(dc == 0), stop=(dc == 7)),
              r=[bh, bWtm], w=[B("bk", kk)])
        yield
        A(lambda e: e.activation(out=qk_[:, 0:512], in_=bank(kq), func=AF.Copy), r=[B("bk", kq)], w=[bqk])
        V(lambda e: e.tensor_copy(out=qk_[:, 512:576], in_=bank(kk)[:, 0:64]), r=[B("bk", kk)], w=[bqk])
        V(lambda e: e.tensor_copy(out=v_aug[:, i, 0:64], in_=bank(kk)[:, 64:128]), r=[B("bk", kk)], w=[B("v", i)])
        V(lambda e: e.tensor_copy(out=widx[:, i, :], in_=bank(kk)[:, 128:132]), r=[B("bk", kk)], w=[B("widx", i)])
        yield
        V(lambda e: e.tensor_tensor(out=sq_[:], in0=qk_[:], in1=qk_[:], op=ALU.mult), r=[bqk], w=[bsq])
        V(lambda e: e.tensor_reduce(out=SSQ_, in_=sq_[:].rearrange("p (a b) -> p a b", b=64), axis=AX.X, op=ALU.add), r=[bsq], w=[bv_])
        V(lambda e: e.tensor_scalar(out=VQ_, in0=SSQ_, scalar1=1.0 / 64, scalar2=EPS, op0=ALU.mult, op1=ALU.add), r=[bv_], w=[bv_])
        yield
        P(lambda e: e.tensor_tensor(out=RQ_, in0=VQ_, in1=NH16[:, 0:9], op=ALU.pow), r=[bv_, B("nhalf")], w=[bv_])
        yield
        V(lambda e: e.tensor_tensor(out=qk_[:].rearrange("p (a b) -> p a b", b=64), in0=qk_[:].rearrange("p (a b) -> p a b", b=64),
                                    in1=RQ_[:, :, None].to_broadcast([128, 9, 64]), op=ALU.mult), r=[bqk, bv_], w=[bqk])
        V(lambda e: e.tensor_tensor(out=qkn_[:], in0=qk_[:], in1=qkw[:], op=ALU.mult), r=[bqk, B("qkw")], w=[bqkn])
        yield
        tq = bankbf(ktq).rearrange("p (a b) -> p a b", b=128)
        tk = bankbf(ktk)[:, 512:640]
        for h in range(8):
            T(lambda e, h=h: e.transpose(tq[0:64, h, :], qkn_[:, h * 64:(h + 1) * 64], identb[:]), r=[bqkn, B("identb")], w=[B("bk", ktq)])
        T(lambda e: e.transpose(tk[0:64, 0:128], qkn_[:, 512:576], identb[:]), r=[bqkn, B("identb")], w=[B("bk", ktk)])
        yield
        A(lambda e: e.activation(out=qTa[0:64, :, i * 128:(i + 1) * 128], in_=tq[0:64, :, :], func=AF.Copy), r=[B("bk", ktq)], w=[B("qT", i)])
        V(lambda e: e.tensor_copy(out=kTa[0:64, i * 128:(i + 1) * 128], in_=tk[0:64, 0:128]), r=[B("bk", ktk)], w=[B("kT", i)])
        yield

    def a12_tile(i):
        yield from a1_tile(i)
        yield from a2_tile(i)

    staggered(a12_tile, NT, depth=2)
    if stop == "A2":
        finish([("qTa", qTa[0:67], [67, 8, SEQ], BF16), ("kTa", kTa[0:67], [67, SEQ], BF16), ("v_aug", v_aug[:], [128, NT, 66], BF16),
                ("widx", widx[:], [128, NT, 4], F32), ("gate_m", gate_m[:], [128, D], F32), ("gate_f", gate_f[:], [128, D], F32), ("sm", sm[:], [128, 256], F32)])
        return
    fm_n = [0]
    bk_n = [0]

    def next_bank():
        k = bk_n[0] % 8
        bk_n[0] += 1
        return k

    def fm_weights(c0, w, place=0, zero=False, slot=None):
        if slot is None:
            slot = fm_n[0] % 4
            fm_n[0] += 1
        bw = B("wfm", slot)
        if zero:
            P(lambda e: e.memset(wfm[slot][:], 0.0), w=[bw])
        load_cast(wfm[slot][:, :, place:place + w], bw, c0, w)
        return slot

    def fm_mm(slot, tb, k):
        for dc in range(8):
            T(lambda e, dc=dc: e.matmul(bank(k), lhsT=wfm[slot][:, dc, :], rhs=hT[:, dc, tb * 512:(tb + 1) * 512], start=(dc == 0), stop=(dc == 7)),
              r=[B("wfm", slot)] + [B("hT", 4 * tb + j) for j in range(4)], w=[B("bk", k)])

    for var in range(2):
        slot = fm_weights(896, 64, place=64 * var, zero=True)
        for tb in range(4):
            k = next_bank()
            fm_mm(slot, tb, k)
            o = kiT[:, var, tb * 512:(tb + 1) * 512]
            if tb % 2 == 0:
                A(lambda e, o=o, k=k: e.activation(out=o, in_=bank(k), func=AF.Copy), r=[B("bk", k)], w=[B("kiT", tb)])
            else:
                V(lambda e, o=o, k=k: e.tensor_copy(out=o, in_=bank(k)), r=[B("bk", k)], w=[B("kiT", tb)])
    for ch in range(2):
        slot = fm_weights(640 + 128 * ch, 128)
        for tb in range(4):
            k = next_bank()
            fm_mm(slot, tb, k)
            o = qiT[:, ch, tb * 512:(tb + 1) * 512]
            if tb % 2 == 0:
                A(lambda e, o=o, k=k: e.activation(out=o, in_=bank(k), func=AF.Copy), r=[B("bk", k)], w=[B("qiT", tb)])
            else:
                V(lambda e, o=o, k=k: e.tensor_copy(out=o, in_=bank(k)), r=[B("bk", k)], w=[B("qiT", tb)])

    S.barrier()
    L.off = Lmark
    wfm.append(L.alloc("wfm4", [128, 8, 128], BF16))
    wfm.append(L.alloc("wfm5", [128, 8, 128], BF16))
    lset = []
    for st_ in range(2):
        d_ = dict(xraw=L.alloc(f"xraw{st_}", [128, 516], F32), xc=L.alloc(f"xc{st_}", [128, 512], F32), xcb=L.alloc(f"xcb{st_}", [128, 512], BF16),
                  t_r=L.alloc(f"t_r{st_}", [128, 512], F32), t_i=L.alloc(f"t_i{st_}", [128, 512], F32),
                  om=L.alloc(f"om{st_}", [128, 512], F32), hb=[L.alloc(f"hb{st_}_{i}", [128, 512], F32) for i in range(2)],
                  g_t=L.alloc(f"g_t{st_}", [128, 512], F32), u_t=L.alloc(f"u_t{st_}", [128, 512], F32))
        lset.append(d_)
    A(lambda e: e.activation(out=SPT, in_=cols[:, 84:88], func=AF.Exp, scale=-1.0), r=[B("cols")], w=[B("spt")])
    A(lambda e: e.activation(out=SPT, in_=SPT, func=AF.Ln, bias=1.0), r=[B("spt")], w=[B("spt")])
    V(lambda e: e.tensor_scalar(out=CA, in0=SPT, scalar1=-4.0, scalar2=None, op0=ALU.mult), r=[B("spt")], w=[B("ca")])
    V(lambda e: e.tensor_scalar(out=HBT, in0=cols[:, 76:80], scalar1=0.5, scalar2=None, op0=ALU.mult), r=[B("cols")], w=[B("hbt")])
    V(lambda e: e.tensor_scalar(out=HBI, in0=cols[:, 80:84], scalar1=0.5, scalar2=None, op0=ALU.mult), r=[B("cols")], w=[B("hbi")])

    lru_wdone = set()

    def lru_chunk(c):
        st_ = c % 2
        d_ = lset[st_]
        xraw, xc, xcb, t_r, t_i, om, hb, g_t, u_t = (d_[k] for k in ("xraw", "xc", "xcb", "t_r", "t_i", "om", "hb", "g_t", "u_t"))
        gg = g_t
        ix = t_i
        a_t = t_r
        nm = lambda k: B(k, st_)
        def lru_w(cc):
            if cc not in lru_wdone and cc < 4:
                lru_wdone.add(cc)
                pr = cc % 3
                fm_weights(964 + 128 * cc, 128, slot=2 * pr)
                fm_weights(1476 + 128 * cc, 128, slot=2 * pr + 1)
        lru_w(c)
        lru_w(c + 1)
        sx, sg = 2 * (c % 3), 2 * (c % 3) + 1
        V(lambda e: e.memset(xraw[:, 0:3], 0.0), w=[nm("xraw")])
        yield
        for tb in range(4):
            kx, kg, kr, ki = next_bank(), next_bank(), next_bank(), next_bank()
            fm_mm(sx, tb, kx)
            yield
            fm_mm(sg, tb, kg)
            yield
            A(lambda e: e.activation(out=xraw[:, 3:515], in_=bank(kx), func=AF.Copy), r=[B("bk", kx)], w=[nm("xraw")])
            A(lambda e: e.activation(out=g_t[:], in_=bank(kg), func=AF.Copy), r=[B("bk", kg)], w=[nm("g_t")])
            yield
            V(lambda e: e.tensor_scalar(out=xc[:], in0=xraw[:, 0:512], scalar1=cols[:, 56 + c:57 + c], scalar2=cols[:, 72 + c:73 + c], op0=ALU.mult, op1=ALU.add),
              r=[nm("xraw"), B("cols")], w=[nm("xc")])
            for j in range(1, 4):
                V(lambda e, j=j: e.scalar_tensor_tensor(out=xc[:], in0=xraw[:, j:j + 512], scalar=cols[:, 56 + 4 * j + c:57 + 4 * j + c], in1=xc[:], op0=ALU.mult, op1=ALU.add),
                  r=[nm("xraw"), B("cols"), nm("xc")], w=[nm("xc")])
            V(lambda e: e.tensor_copy(out=xraw[:, 0:3], in_=xraw[:, 512:515]), r=[nm("xraw")], w=[nm("xraw")])
            yield
            P(lambda e: e.tensor_copy(out=xcb[:], in_=xc[:]), r=[nm("xc")], w=[nm("xcb")])
            P(lambda e: e.tensor_tensor(out=u_t[:], in0=g_t[:], in1=g_t[:], op=ALU.mult), r=[nm("g_t")], w=[nm("u_t")])
            yield
            T(lambda e: e.matmul(bank(kr), lhsT=wbd_b[:, 0, c, :], rhs=xcb[:], start=True, stop=True), r=[B("wbd_b"), nm("xcb")], w=[B("bk", kr)])
            T(lambda e: e.matmul(bank(ki), lhsT=wbd_b[:, 1, c, :], rhs=xcb[:], start=True, stop=True), r=[B("wbd_b"), nm("xcb")], w=[B("bk", ki)])
            V(lambda e: e.tensor_scalar(out=u_t[:], in0=u_t[:], scalar1=0.044715, scalar2=1.0, op0=ALU.mult, op1=ALU.add), r=[nm("u_t")], w=[nm("u_t")])
            yield
            A(lambda e: e.activation(out=t_r[:], in_=bank(kr), func=AF.Tanh, scale=0.5, bias=HBT[:, c:c + 1]), r=[B("bk", kr), B("hbt")], w=[nm("t_r")])
            A(lambda e: e.activation(out=t_i[:], in_=bank(ki), func=AF.Tanh, scale=0.5, bias=HBI[:, c:c + 1]), r=[B("bk", ki), B("hbi")], w=[nm("t_i")])
            P(lambda e: e.tensor_tensor(out=u_t[:], in0=u_t[:], in1=g_t[:], op=ALU.mult), r=[nm("u_t"), nm("g_t")], w=[nm("u_t")])
            yield
            A(lambda e: e.activation(out=a_t[:], in_=t_r[:], func=AF.Exp, scale=CA[:, c:c + 1], bias=CA[:, c:c + 1]), r=[nm("t_r"), B("ca")], w=[nm("t_r")])
            A(lambda e: e.activation(out=u_t[:], in_=u_t[:], func=AF.Tanh, scale=0.7978845608028654), r=[nm("u_t")], w=[nm("u_t")])
            V(lambda e: e.scalar_tensor_tensor(out=ix[:], in0=t_i[:], scalar=1.0, in1=xc[:], op0=ALU.add, op1=ALU.mult), r=[nm("t_i"), nm("xc")], w=[nm("t_i")])
            yield
            P(lambda e: e.tensor_tensor(out=om[:], in0=a_t[:], in1=a_t[:], op=ALU.mult), r=[nm("t_r")], w=[nm("om")])
            V(lambda e: e.scalar_tensor_tensor(out=gg[:], in0=u_t[:], scalar=1.0, in1=g_t[:], op0=ALU.add, op1=ALU.mult), r=[nm("u_t"), nm("g_t")], w=[nm("g_t")])
            yield
            A(lambda e: e.activation(out=om[:], in_=om[:], func=AF.Sqrt, scale=-1.0, bias=1.0), r=[nm("om")], w=[nm("om")])
            yield
            P(lambda e: e.tensor_tensor(out=ix[:], in0=ix[:], in1=om[:], op=ALU.mult), r=[nm("t_i"), nm("om")], w=[nm("t_i")])
            yield
            hs = tb % 2
            init = 0.0 if tb == 0 else hb[1 - hs][:, 511:512]
            V(lambda e, hs=hs, init=init: e.tensor_tensor_scan(out=hb[hs][:], data0=a_t[:], data1=ix[:], initial=init, op0=ALU.mult, op1=ALU.add),
              r=[nm("t_r"), nm("t_i"), B("hb", st_, 1 - hs)], w=[B("hb", st_, hs)])
            yield
            V(lambda e, hs=hs: e.scalar_tensor_tensor(out=hgT[:, c, tb * 512:(tb + 1) * 512], in0=hb[hs][:], scalar=0.25, in1=gg[:], op0=ALU.mult, op1=ALU.mult),
              r=[B("hb", st_, hs), nm("g_t")], w=[B("hgT", tb)])
            yield

    def ada_bg():
        for p_ in range(4, 12):
            ada_dma(p_, slot=0)
            for _ in range(12):
                yield
            ada_mm(p_, kb=next_bank())
            yield
        ada_finish(1)

    interleave(staggered_g(lru_chunk, 4), ada_bg())

    S.barrier()
    if stop == "A3":
        finish([("qiT", qiT[:], [128, 2, SEQ], BF16), ("kiT", kiT[:], [128, 2, SEQ], BF16), ("hgT", hgT[:], [128, 4, SEQ], BF16)])
        return

    L.reset()
    score = [L.alloc(f"score{i}", [128, SEQ], F32) for i in range(3)]
    mb = [L.alloc(f"mb{i}", [128, SEQ], BF16) for i in range(2)]
    junk = L.alloc("junk", [128, SEQ], BF16)
    rtmp = [L.alloc(f"rtmp{i}", [128, 256], F32) for i in range(4)]
    gehi = L.alloc("gehi", [128, SEQ], BF16)
    junkA = L.alloc("junkA", [128, SEQ], BF16)
    bandb = L.alloc("bandb", [128, SEQ], BF16)
    nstp = L.alloc("nstp", [128, 3, NBIS], F32)
    cum = L.alloc("cum", [128, SEQ], F32)
    onesb = L.alloc("onesb", [128, SEQ], BF16)
    V(lambda e: e.memset(onesb[:], 1.0), w=[B("onesb")])
    PT = [L.alloc(f"PT{i}", [128, 512], BF16) for i in range(3)]
    attn_tm = L.alloc("attn_tm", [128, 512], BF16)
    rs = L.alloc("rs", [128, 8], F32)
    stp = L.alloc("stp", [128, 3, NBIS], F32)
    cvec = L.alloc("cvec", [128, NBIS], F32)
    for k in range(NBIS):
        V(lambda e, k=k: e.memset(cvec[:, k:k + 1], 2.0 ** -(k + 1)), w=[B("cvec")])

    def _hdr(i):
        par = i % 3
        Lk = 128 * (i + 1)
        sc = score[i % 3]
        bs = B("score", i % 3)
        LO = BIS[:, par * 16 + 0:par * 16 + 1]
        W0 = BIS[:, par * 16 + 1:par * 16 + 2]
        MID = BIS[:, par * 16 + 2:par * 16 + 3]
        CNT = BIS[:, par * 16 + 3:par * 16 + 4]
        FS = BIS[:, par * 16 + 4:par * 16 + 5]
        AM = BIS[:, par * 16 + 5:par * 16 + 6]
        bb = B("bis", par)
        return par, Lk, sc, bs, LO, W0, MID, CNT, FS, AM, bb

    def idx_scores(i):
        par, Lk, sc, bs, LO, W0, MID, CNT, FS, AM, bb = _hdr(i)
        if i < 2:
            V(lambda e: e.memset(sc[:, 0:Lk], 0.0), w=[bs])
            V(lambda e: e.memset(sc[0:64, Lk - 64:Lk], SENT), w=[bs])
            V(lambda e: e.memset(LO, -1.0), w=[bb])
            yield
        else:
            nblk = (Lk + 255) // 256
            for h in range(4):
                for bl in range(nblk):
                    half = bl % 2
                    c0 = bl * 256
                    cw_ = min(256, Lk - c0)
                    pidx = bank(half)[:, 0:cw_]
                    T(lambda e, h=h, c0=c0, cw_=cw_, pidx=pidx: e.matmul(pidx, lhsT=qiT[:, h // 2, i * 128:(i + 1) * 128], rhs=kiT[:, h % 2, c0:c0 + cw_], start=True, stop=True),
                      r=[B("qiT", i // 4), B("kiT", bl // 2)], w=[B("bk", half)])
                    rt = rtmp[(h * nblk + bl) % 4]
                    brt = B("rtmp", (h * nblk + bl) % 4)
                    dst = sc[:, c0:c0 + cw_]
                    if h == 0:
                        V(lambda e, pidx=pidx, dst=dst: e.tensor_scalar(out=dst, in0=pidx, scalar1=0.0, scalar2=widx[:, i, 0:1], op0=ALU.max, op1=ALU.mult),
                          r=[B("bk", half), B("widx", i)], w=[bs])
                    else:
                        V(lambda e, pidx=pidx, rt=rt, h=h, cw_=cw_: e.tensor_scalar(out=rt[:, 0:cw_], in0=pidx, scalar1=0.0, scalar2=widx[:, i, h:h + 1], op0=ALU.max, op1=ALU.mult),
                          r=[B("bk", half), B("widx", i)], w=[brt])
                        P(lambda e, rt=rt, dst=dst, cw_=cw_: e.tensor_tensor(out=dst, in0=dst, in1=rt[:, 0:cw_], op=ALU.add), r=[brt, bs], w=[bs])
                    yield
            V(lambda e: e.tensor_reduce(out=AM, in_=sc[:, 0:Lk], axis=AX.X, op=ALU.max, apply_absolute_value=True), r=[bs], w=[bb])
            V(lambda e: e.tensor_scalar(out=LO, in0=AM, scalar1=-1.001, scalar2=-1e-20, op0=ALU.mult, op1=ALU.add), r=[bb], w=[bb])
            V(lambda e: e.tensor_scalar(out=W0, in0=LO, scalar1=-2.0, scalar2=None, op0=ALU.mult), r=[bb], w=[bb])
            V(lambda e: e.tensor_tensor(out=stp[:, par, :], in0=cvec[:], in1=W0.to_broadcast([128, NBIS]), op=ALU.mult), r=[bb, B("cvec")], w=[B("stp", par)])
            V(lambda e: e.memset(sc[0:64, Lk - 64:Lk], SENT), r=[bs], w=[bs])
            yield

    def bisect(i):
        par, Lk, sc, bs, LO, W0, MID, CNT, FS, AM, bb = _hdr(i)
        if i >= 2:
            SL = stp[:, par, NBIS - 1:NBIS]
            yield
            if i < ACT_BIS_FROM:
                V(lambda e: e.tensor_tensor(out=MID, in0=LO, in1=stp[:, par, 0:1], op=ALU.add), r=[bb, B("stp", par)], w=[bb])
                for k in range(NBIS):
                    V(lambda e: e.tensor_scalar(out=junk[:, 0:Lk], in0=sc[:, 0:Lk], scalar1=MID, scalar2=None, op0=ALU.is_ge, op1=ALU.add, accum_out=CNT),
                      r=[bs, bb], w=[B("junk"), bb])
                    V(lambda e: e.tensor_scalar(out=FS, in0=CNT, scalar1=255.5, scalar2=0.5, op0=ALU.is_ge, op1=ALU.subtract), r=[bb], w=[bb])
                    if k + 1 < NBIS:
                        V(lambda e, k=k: e.scalar_tensor_tensor(out=MID, in0=FS, scalar=stp[:, par, k:k + 1], in1=MID, op0=ALU.mult, op1=ALU.add), r=[bb, B("stp", par)], w=[bb])
                    yield
                V(lambda e: e.tensor_scalar(out=FS, in0=FS, scalar1=-0.5, scalar2=None, op0=ALU.add), r=[bb], w=[bb])
                V(lambda e: e.scalar_tensor_tensor(out=LO, in0=FS, scalar=SL, in1=MID, op0=ALU.mult, op1=ALU.add), r=[bb, B("stp", par)], w=[bb])
            else:
                NMID = BIS[:, par * 16 + 9:par * 16 + 10]
                SSUM = BIS[:, par * 16 + 10:par * 16 + 11]
                SG = BIS[:, par * 16 + 11:par * 16 + 12]
                ba = B("bisA", par)
                V(lambda e: e.tensor_scalar(out=nstp[:, par, :], in0=stp[:, par, :], scalar1=-1.0, scalar2=None, op0=ALU.mult), r=[B("stp", par)], w=[B("nstp", par)])
                V(lambda e: e.scalar_tensor_tensor(out=NMID, in0=LO, scalar=-1.0, in1=nstp[:, par, 0:1], op0=ALU.mult, op1=ALU.add), r=[bb, B("nstp", par)], w=[ba])
                for k in range(NBIS):
                    A(lambda e: e.activation(out=junkA[:, 0:Lk], in_=sc[:, 0:Lk], func=AF.Sign, bias=NMID, scale=1.0, accum_out=SSUM), r=[bs, ba], w=[B("junkA"), ba])
                    A(lambda e: e.activation(out=SG, in_=SSUM, func=AF.Sign, bias=float(Lk - 512) + 0.5), r=[ba], w=[ba])
                    if k + 1 < NBIS:
                        A(lambda e, k=k: e.activation(out=NMID, in_=SG, func=AF.Identity, scale=nstp[:, par, k + 1:k + 2], bias=NMID), r=[ba, B("nstp", par)], w=[ba])
                    yield
                V(lambda e: e.tensor_scalar(out=FS, in0=SG, scalar1=0.5, scalar2=-0.5, op0=ALU.mult, op1=ALU.add), r=[ba], w=[bb])
                V(lambda e: e.scalar_tensor_tensor(out=LO, in0=FS, scalar=SL, in1=NMID, op0=ALU.mult, op1=ALU.subtract), r=[bb, ba, B("stp", par)], w=[bb])
        yield

    def bandsel(i):
        par, Lk, sc, bs, LO, W0, MID, CNT, FS, AM, bb = _hdr(i)
        if i < 2:
            V(lambda e: e.tensor_scalar(out=mb[i % 2][:, 0:Lk], in0=sc[:, 0:Lk], scalar1=LO, scalar2=NEG, op0=ALU.is_lt, op1=ALU.mult), r=[bs, bb], w=[B("mb", i % 2)])
        else:
            HI = BIS[:, par * 16 + 6:par * 16 + 7]
            CHI = BIS[:, par * 16 + 7:par * 16 + 8]
            NKEEP = BIS[:, par * 16 + 8:par * 16 + 9]
            V(lambda e: e.tensor_tensor(out=HI, in0=LO, in1=stp[:, par, NBIS - 1:NBIS], op=ALU.add), r=[bb, B("stp", par)], w=[bb])
            V(lambda e: e.tensor_scalar(out=gehi[:, 0:Lk], in0=sc[:, 0:Lk], scalar1=HI, scalar2=None, op0=ALU.is_ge, op1=ALU.add, accum_out=CHI), r=[bs, bb], w=[B("gehi"), bb])
            yield
            V(lambda e: e.tensor_scalar(out=NKEEP, in0=CHI, scalar1=-1.0, scalar2=256.0, op0=ALU.mult, op1=ALU.add), r=[bb], w=[bb])
            V(lambda e: e.scalar_tensor_tensor(out=bandb[:, 0:Lk], in0=sc[:, 0:Lk], scalar=LO, in1=gehi[:, 0:Lk], op0=ALU.is_ge, op1=ALU.subtract), r=[bs, bb, B("gehi")], w=[B("bandb")])
            yield
            V(lambda e: e.tensor_tensor_scan(out=cum[:, 0:Lk], data0=onesb[:, 0:Lk], data1=bandb[:, 0:Lk], initial=0.0, op0=ALU.mult, op1=ALU.add), r=[B("bandb"), B("onesb")], w=[B("cum")])
            yield
            V(lambda e: e.scalar_tensor_tensor(out=bandb[:, 0:Lk], in0=cum[:, 0:Lk], scalar=NKEEP, in1=bandb[:, 0:Lk], op0=ALU.is_le, op1=ALU.mult), r=[B("cum"), bb, B("bandb")], w=[B("bandb")])
            yield
            P(lambda e: e.tensor_scalar(out=gehi[:, 0:Lk], in0=gehi[:, 0:Lk], scalar1=-NEG, scalar2=NEG, op0=ALU.mult, op1=ALU.add), r=[B("gehi"), B("bandb")], w=[B("gehi")])
            V(lambda e: e.scalar_tensor_tensor(out=mb[i % 2][:, 0:Lk], in0=bandb[:, 0:Lk], scalar=-NEG, in1=gehi[:, 0:Lk], op0=ALU.mult, op1=ALU.add), r=[B("bandb"), B("gehi")], w=[B("mb", i % 2)])

    unit_ctr = [0]

    def attention(i):
        par = i % 2
        nk = i + 1
        units = []
        for h in range(8):
            for u0 in range(0, nk, 4):
                units.append((h, u0, min(nk, u0 + 4)))
        pend = None
        rd_q = [B("qT", i), B("qTa_aug"), B("kTa_aug")] + [B("kT", j) for j in range(nk)]

        def emit_qk(h, u0, u1, uidx):
            psu = bank(2 + uidx % 3)
            bps = B("bk", 2 + uidx % 3)
            for j in range(u0, u1):
                cs = (j - u0) * 128
                T(lambda e, j=j, cs=cs: e.matmul(psu[:, cs:cs + 128], lhsT=kTa[0:67, j * 128:(j + 1) * 128], rhs=qTa[0:67, h, i * 128:(i + 1) * 128], start=True, stop=False),
                  r=rd_q, w=[bps])
                T(lambda e, j=j, cs=cs: e.matmul(psu[:, cs:cs + 128], lhsT=mb[par][:, j * 128:(j + 1) * 128], rhs=identb[:], start=False, stop=(j != i)),
                  r=[B("mb", par), B("identb")], w=[bps])
                if j == i:
                    T(lambda e, cs=cs: e.matmul(psu[:, cs:cs + 128], lhsT=identb[:], rhs=dmat[:, h, :], start=False, stop=True),
                      r=[B("dmat"), B("identb")], w=[bps])
            slot = uidx % 3
            ncol = (u1 - u0) * 128
            A(lambda e: e.activation(out=PT[slot][:, 0:ncol], in_=psu[:, 0:ncol], func=AF.Exp), r=[bps], w=[B("PT", slot)])

        def emit_pv(h, u0, u1, uidx):
            slot = uidx % 3
            pv = bank(6 + h // 4)
            hc = (h % 4) * 65
            for j in range(u0, u1):
                cs = (j - u0) * 128
                T(lambda e, j=j, cs=cs: e.matmul(pv[:, hc:hc + 65], lhsT=PT[slot][:, cs:cs + 128], rhs=v_aug[:, j, 0:65], start=(j == 0), stop=(j == i)),
                  r=[B("PT", slot), B("v", j), B("v_ones")], w=[B("bk", 6 + h // 4)])

        for (h, u0, u1) in units:
            uidx = unit_ctr[0]
            unit_ctr[0] += 1
            emit_qk(h, u0, u1, uidx)
            if pend is not None:
                emit_pv(*pend)
            pend = (h, u0, u1, uidx)
            yield
        emit_pv(*pend)
        yield
        for g in range(2):
            pv3 = bank(6 + g)[:, 0:260].rearrange("p (a b) -> p a b", b=65)
            V(lambda e, g=g, pv3=pv3: e.tensor_scalar(out=rs[:, g * 4:(g + 1) * 4], in0=pv3[:, :, 64], scalar1=1e-30, scalar2=None, op0=ALU.add), r=[B("bk", 6 + g)], w=[B("rs")])
        V(lambda e: e.reciprocal(out=rs[:], in_=rs[:]), r=[B("rs")], w=[B("rs")])
        for g in range(2):
            pv3 = bank(6 + g)[:, 0:260].rearrange("p (a b) -> p a b", b=65)
            V(lambda e, g=g, pv3=pv3: e.tensor_tensor(out=attn_tm[:, g * 256:(g + 1) * 256].rearrange("p (a b) -> p a b", b=64), in0=pv3[:, :, 0:64],
                                                   in1=rs[:, g * 4:(g + 1) * 4, None].to_broadcast([128, 4, 64]), op=ALU.mult),
              r=[B("bk", 6 + g), B("rs")], w=[B("attn_tm")])
        tp = bankbf(5).rearrange("p (a b) -> p a b", b=128)
        for cc in range(4):
            T(lambda e, cc=cc: e.transpose(tp[:, cc, :], attn_tm[:, cc * 128:(cc + 1) * 128], identb[:]), r=[B("attn_tm"), B("identb")], w=[B("bk", 5)])
        A(lambda e: e.activation(out=attnT[:, :, i * 128:(i + 1) * 128], in_=tp[:, 0:4, :], func=AF.Copy), r=[B("bk", 5)], w=[B("attnT", i)])

    g_ = lambda f, k: f(k) if 0 <= k < NT else None
    interleave(idx_scores(0))
    interleave(bisect(0), idx_scores(1))
    interleave(bandsel(0), bisect(1), idx_scores(2))
    for i in range(NT):
        interleave(attention(i), g_(bandsel, i + 1), g_(bisect, i + 2), g_(idx_scores, i + 3))

    S.barrier()
    if stop == "B":
        finish([("attnT", attnT[:], [128, 4, SEQ], BF16), ("mb1", mb[1][:], [128, SEQ], BF16),
                ("score1", score[0][:], [128, SEQ], F32)])
        return

    M2.reset()
    L.reset()
    mergedT = M2.alloc("mergedT", [128, 8, SEQ], BF16)
    Wout = M2.alloc("Wout", [128, 8, D], BF16)
    wg = [M2.alloc(f"wg{i}", [128, 8, 256], BF16) for i in range(2)]
    h2T = L.alloc("h2T", [128, 8, SEQ], BF16)
    comb = L.alloc("comb", [128, NT, 32], F32)
    Lkeep = L.off
    wrt = L.alloc("wrt", [128, 8, 36], F32)
    logits = L.alloc("logits", [128, NT, 36], F32)
    Lov = L.off
    wp = [L.alloc(f"wp{i}", [128, 4, 256], BF16) for i in range(2)]
    wst = [L.alloc(f"cwst{i}", [128, 8, 128], F32) for i in range(2)]
    sa = L.alloc("sa", [128, 512], F32)
    sb_ = L.alloc("sb_", [128, 512], F32)
    m1 = L.alloc("m1", [128, 512], F32)
    m2 = L.alloc("m2", [128, 512], F32)
    L.off = Lov
    xin = [L.alloc(f"cxin{i}", [128, D], F32) for i in range(3)]
    sqj2 = L.alloc("sqj2", [128, D], BF16)
    h2fs = [L.alloc(f"h2f{i}", [128, 8, 128], F32) for i in range(3)]
    lgTs = [L.alloc(f"lgT{i}", [128, 128], F32) for i in range(2)]
    x1 = nc.alloc_sbuf_tensor_at("x1", [128, NT, D], F32, offset=BASE + 13 * 1024)

    wpa_v = wpa_d.rearrange("(kc p) n -> p kc n", p=128)
    wpb_v = wpb_d.rearrange("(kc p) n -> p kc n", p=128)
    wout_v = wout_d.rearrange("(kc p) n -> p kc n", p=128)
    S.dma("pool", wrt[:], wrt_d.rearrange("(kc p) n -> p kc n", p=128), writes=[B("wrt")])

    def c1_loads(n):
        ws = n % 2
        bwg, bwp = B("wg", ws), B("wp", ws)
        load_cast(wg[ws][:, :, 0:128], bwg, 1988 + 128 * n, 128)
        load_cast(wg[ws][:, :, 128:256], bwg, 3012 + 128 * n, 128)
        load_cast(wp[ws][:, :, 0:128], bwp, 128 * n, 128, dram_v=wpa_v)
        load_cast(wp[ws][:, :, 128:256], bwp, 128 * n, 128, dram_v=wpb_v)

    c1_loads(0)
    for n in range(8):
        ws = n % 2
        bwg, bwp = B("wg", ws), B("wp", ws)
        if n + 1 < 8:
            c1_loads(n + 1)
        if n == 1:
            for q4 in range(8):
                slot = wst_n[0] % 2
                wst_n[0] += 1
                st = wst[slot]
                bst = B("wst", slot)
                S.dma("sp", st[:], wout_v[:, :, q4 * 128:(q4 + 1) * 128], writes=[bst])
                P(lambda e, st=st, q4=q4: e.tensor_tensor(out=Wout[:, :, q4 * 128:(q4 + 1) * 128], in0=st[:], in1=gate_m[:, q4 * 128:(q4 + 1) * 128].rearrange("p (o n) -> p o n", o=1).to_broadcast([128, 8, 128]), op=ALU.mult),
                  r=[bst, B("gate", 2)], w=[B("Wout")])
        for tb in range(4):
            base = 4 * ((n * 4 + tb) % 2)
            kya, kyb, kga, kgb = base, base + 1, base + 2, base + 3
            cs = slice(tb * 512, (tb + 1) * 512)
            rdh = [B("hT", 4 * tb + j) for j in range(4)]
            for kc in range(4):
                T(lambda e, kc=kc: e.matmul(bank(kya), lhsT=wp[ws][:, kc, 0:128], rhs=attnT[:, kc, cs], start=(kc == 0), stop=(kc == 3)),
                  r=[bwp] + [B("attnT", 4 * tb + j) for j in range(4)], w=[B("bk", kya)])
            for kc in range(4):
                T(lambda e, kc=kc: e.matmul(bank(kyb), lhsT=wp[ws][:, kc, 128:256], rhs=hgT[:, kc, cs], start=(kc == 0), stop=(kc == 3)),
                  r=[bwp, B("hgT", tb)], w=[B("bk", kyb)])
            for kc in range(8):
                T(lambda e, kc=kc: e.matmul(bank(kga), lhsT=wg[ws][:, kc, 0:128], rhs=hT[:, kc, cs], start=(kc == 0), stop=(kc == 7)), r=[bwg] + rdh, w=[B("bk", kga)])
            for kc in range(8):
                T(lambda e, kc=kc: e.matmul(bank(kgb), lhsT=wg[ws][:, kc, 128:256], rhs=hT[:, kc, cs], start=(kc == 0), stop=(kc == 7)), r=[bwg] + rdh, w=[B("bk", kgb)])
            A(lambda e: e.activation(out=sa[:], in_=bank(kga), func=AF.Tanh, scale=0.5), r=[B("bk", kga)], w=[B("sa")])
            A(lambda e: e.activation(out=sb_[:], in_=bank(kgb), func=AF.Tanh, scale=0.5), r=[B("bk", kgb)], w=[B("sb")])
            V(lambda e: e.scalar_tensor_tensor(out=m1[:], in0=sa[:], scalar=1.0, in1=bank(kya), op0=ALU.add, op1=ALU.mult), r=[B("sa"), B("bk", kya)], w=[B("m1")])
            V(lambda e: e.scalar_tensor_tensor(out=m2[:], in0=sb_[:], scalar=1.0, in1=bank(kyb), op0=ALU.add, op1=ALU.mult), r=[B("sb"), B("bk", kyb)], w=[B("m2")])
            V(lambda e: e.tensor_tensor(out=m1[:], in0=m1[:], in1=m2[:], op=ALU.add), r=[B("m1"), B("m2")], w=[B("m1")])
            A(lambda e, n=n, cs=cs: e.activation(out=mergedT[:, n, cs], in_=m1[:], func=AF.Identity, scale=0.5), r=[B("m1")], w=[B("mergedT", tb)])

    S.barrier()
    def c2_tile(i):
        sl = i % 2
        s3 = i % 3
        bx = B("cxin", s3)
        xn2 = xin[s3]
        h2f = h2fs[s3]
        bxn, bhf = bx, B("h2f", s3)
        S.dma("pool", xin[s3][:], x_d[i * 128:(i + 1) * 128, :], writes=[bx])
        pso = PS[0]
        bpo = B("PSU", 0)
        for hf in range(2):
            for kc in range(8):
                T(lambda e, kc=kc, hf=hf: e.matmul(pso[:, hf * 512:(hf + 1) * 512], lhsT=mergedT[:, kc, i * 128:(i + 1) * 128], rhs=Wout[:, kc, hf * 512:(hf + 1) * 512], start=(kc == 0), stop=(kc == 7)),
                  r=[B("mergedT", i // 4), B("Wout")], w=[bpo])
        yield
        bx1 = B("x1", i)
        V(lambda e: e.tensor_tensor(out=x1[:, i, :], in0=pso[:, :], in1=xin[s3][:], op=ALU.add), r=[bpo, bx], w=[bx1])
        yield
        A(lambda e: e.activation(out=sqj2[:], in_=x1[:, i, :], func=AF.Square, accum_out=SS2[:, i:i + 1]), r=[bx1], w=[B("sqj2"), B("ss2", i)])
        yield
        V(lambda e: e.tensor_scalar(out=VAR2[:, i:i + 1], in0=SS2[:, i:i + 1], scalar1=1.0 / D, scalar2=EPS, op0=ALU.mult, op1=ALU.add), r=[B("ss2", i)], w=[B("var2", i)])
        P(lambda e: e.tensor_tensor(out=RSTD2[:, i:i + 1], in0=VAR2[:, i:i + 1], in1=NHALF, op=ALU.pow), r=[B("var2", i), B("nhalf")], w=[B("rstd2", i)])
        yield
        V(lambda e: e.tensor_scalar(out=xn2[:], in0=x1[:, i, :], scalar1=RSTD2[:, i:i + 1], scalar2=None, op0=ALU.mult), r=[bx1, B("rstd2", i)], w=[bxn])
        yield
        pst = PS[2 + sl]
        kb0, kb1 = 4 + 2 * sl, 5 + 2 * sl
        for dc in range(8):
            T(lambda e, dc=dc: e.transpose(pst[:, dc * 128:(dc + 1) * 128], xn2[:, dc * 128:(dc + 1) * 128], identf[:]), r=[bxn, B("identf")], w=[B("bk", kb0 + dc // 4)])
        yield
        for dc in range(8):
            if dc < 4:
                A(lambda e, dc=dc: e.activation(out=h2f[:, dc, :], in_=pst[:, dc * 128:(dc + 1) * 128], func=AF.Identity, scale=A_F[:, dc:dc + 1], bias=B_F[:, dc:dc + 1]),
                  r=[B("bk", kb0), B("AB", 1), c_sm], w=[bhf])
            else:
                V(lambda e, dc=dc: e.tensor_scalar(out=h2f[:, dc, :], in0=pst[:, dc * 128:(dc + 1) * 128], scalar1=A_F[:, dc:dc + 1], scalar2=B_F[:, dc:dc + 1], op0=ALU.mult, op1=ALU.add),
                  r=[B("bk", kb1), B("AB", 1), c_sm], w=[bhf])
        yield
        V(lambda e: e.tensor_copy(out=h2T[:, :, i * 128:(i + 1) * 128], in_=h2f[:]), r=[bhf], w=[B("h2T", i)])
        plT = bank(2 + sl)[0:36, 0:128]
        pl = bank(2 + sl)[:, 128:164]
        bkr = B("bk", 2 + sl)
        for dc in range(8):
            T(lambda e, dc=dc: e.matmul(plT, lhsT=wrt[:, dc, :], rhs=h2f[:, dc, :], start=(dc == 0), stop=(dc == 7)), r=[bhf, B("wrt")], w=[bkr])
        yield
        lgT = lgTs[sl]
        A(lambda e: e.activation(out=lgT[0:36, :], in_=plT, func=AF.Copy), r=[bkr], w=[B("lgT", sl)])
        yield
        T(lambda e: e.transpose(pl, lgT[0:36, :], identf[0:36, 0:36]), r=[B("lgT", sl), B("identf")], w=[bkr])
        yield
        V(lambda e: e.tensor_tensor(out=logits[:, i, :], in0=pl, in1=rows[:, 0:36], op=ALU.add), r=[bkr, B("rows")], w=[B("logits")])
        yield

    staggered(c2_tile, NT, depth=3)

    S.barrier()
    M2.reset()
    W13 = [M2.alloc(f"W13_{i}", [128, 8, 512], BF16) for i in range(2)]
    W2b = [M2.alloc(f"W2b_{i}", [128, 2, D], BF16) for i in range(2)]
    mst = [M2.alloc(f"mst{i}", [128, 2048], F32) for i in range(3)]
    mst_n = [0]

    def moe_load(e_):
        ws = e_ % 2
        for which, (src, dst3, isw2) in enumerate([
            (w1_d[e_].rearrange("(kc p) f -> p kc f", p=128), W13[ws][:, :, 0:256], False),
            (w3_d[e_].rearrange("(kc p) f -> p kc f", p=128), W13[ws][:, :, 256:512], False),
            (w2_d[e_].rearrange("(fc p) n -> p fc n", p=128), W2b[ws][:, :, :], True)]):
            slot = mst_n[0] % 3
            mst_n[0] += 1
            bst = B("mst", slot)
            if isw2:
                stv = mst[slot][:, :].rearrange("p (a b) -> p a b", b=D)
                S.dma("sp", stv, src, writes=[bst])
                P(lambda e, stv=stv, dst3=dst3: e.tensor_tensor(out=dst3, in0=stv, in1=gate_f[:, :].rearrange("p (o n) -> p o n", o=1).to_broadcast([128, 2, D]), op=ALU.mult),
                  r=[bst, B("gate", 5)], w=[B("W2b", ws)])
            else:
                stv = mst[slot][:, :].rearrange("p (a b) -> p a b", b=256)
                S.dma("sp", stv, src, writes=[bst])
                P(lambda e, stv=stv, dst3=dst3: e.tensor_copy(out=dst3, in_=stv), r=[bst], w=[B("W13", ws)])

    moe_load(0)
    L.off = Lov
    r8 = L.alloc("r8", [128, NT, 8], F32)
    r8b = L.alloc("r8b", [128, NT, 8], F32)
    r32 = L.alloc("r32", [128, NT, 32], F32)
    r4 = L.alloc("r4", [128, NT, 4], F32)
    goh = L.alloc("goh", [128, NT, 4], F32)
    rv = L.alloc("rv", [128, 8, NT], F32)
    GMAX, GSUM, M1_, M2_, W1_, W2_, GW = (rv[:, k, :] for k in range(7))
    bl_, brt = B("logits"), B("route")
    gl = logits[:, :, 0:4]
    el = logits[:, :, 4:36]
    V(lambda e: e.tensor_reduce(out=GMAX, in_=gl, axis=AX.X, op=ALU.max), r=[bl_], w=[brt])
    V(lambda e: e.tensor_tensor(out=goh[:], in0=gl, in1=GMAX[:, :, None].to_broadcast([128, NT, 4]), op=ALU.is_equal), r=[bl_, brt], w=[brt])
    V(lambda e: e.tensor_tensor(out=r4[:], in0=gl, in1=GMAX[:, :, None].to_broadcast([128, NT, 4]), op=ALU.subtract), r=[bl_, brt], w=[brt])
    A(lambda e: e.activation(out=r4[:], in_=r4[:], func=AF.Exp), r=[brt], w=[brt])
    V(lambda e: e.tensor_reduce(out=GSUM, in_=r4[:], axis=AX.X, op=ALU.add), r=[brt], w=[brt])
    V(lambda e: e.reciprocal(out=GW, in_=GSUM), r=[brt], w=[brt])
    V(lambda e: e.tensor_tensor(out=r32[:].rearrange("p t (g j) -> p t g j", j=8), in0=el.rearrange("p t (g j) -> p t g j", j=8),
                                in1=goh[:, :, :, None].to_broadcast([128, NT, 4, 8]), op=ALU.mult), r=[bl_, brt], w=[brt])
    V(lambda e: e.tensor_reduce(out=r8[:], in_=r32[:].rearrange("p t (g j) -> p t j g", j=8), axis=AX.X, op=ALU.add), r=[brt], w=[brt])
    V(lambda e: e.tensor_reduce(out=M1_, in_=r8[:], axis=AX.X, op=ALU.max), r=[brt], w=[brt])
    oh1 = r8b
    V(lambda e: e.tensor_tensor(out=oh1[:], in0=r8[:], in1=M1_[:, :, None].to_broadcast([128, NT, 8]), op=ALU.is_equal), r=[brt], w=[brt])
    e2 = L.alloc("e2", [128, NT, 8], F32)
    V(lambda e: e.scalar_tensor_tensor(out=e2[:], in0=oh1[:], scalar=SENT, in1=r8[:], op0=ALU.mult, op1=ALU.add), r=[brt], w=[brt])
    V(lambda e: e.tensor_reduce(out=M2_, in_=e2[:], axis=AX.X, op=ALU.max), r=[brt], w=[brt])
    oh2 = L.alloc("oh2", [128, NT, 8], F32)
    V(lambda e: e.tensor_tensor(out=oh2[:], in0=e2[:], in1=M2_[:, :, None].to_broadcast([128, NT, 8]), op=ALU.is_equal), r=[brt], w=[brt])
    V(lambda e: e.tensor_tensor(out=W2_, in0=M2_, in1=M1_, op=ALU.subtract), r=[brt], w=[brt])
    A(lambda e: e.activation(out=W2_, in_=W2_, func=AF.Exp), r=[brt], w=[brt])
    V(lambda e: e.tensor_scalar(out=W1_, in0=W2_, scalar1=1.0, scalar2=None, op0=ALU.add), r=[brt], w=[brt])
    V(lambda e: e.reciprocal(out=W1_, in_=W1_), r=[brt], w=[brt])
    V(lambda e: e.tensor_tensor(out=W2_, in0=W2_, in1=W1_, op=ALU.mult), r=[brt], w=[brt])
    V(lambda e: e.tensor_tensor(out=W1_, in0=W1_, in1=GW, op=ALU.mult), r=[brt], w=[brt])
    V(lambda e: e.tensor_tensor(out=W2_, in0=W2_, in1=GW, op=ALU.mult), r=[brt], w=[brt])
    V(lambda e: e.tensor_tensor(out=oh1[:], in0=oh1[:], in1=W1_[:, :, None].to_broadcast([128, NT, 8]), op=ALU.mult), r=[brt], w=[brt])
    V(lambda e: e.tensor_tensor(out=oh2[:], in0=oh2[:], in1=W2_[:, :, None].to_broadcast([128, NT, 8]), op=ALU.mult), r=[brt], w=[brt])
    V(lambda e: e.tensor_tensor(out=oh1[:], in0=oh1[:], in1=oh2[:], op=ALU.add), r=[brt], w=[brt])
    for g in range(4):
        V(lambda e, g=g: e.tensor_tensor(out=comb[:, :, g * 8:(g + 1) * 8], in0=oh1[:], in1=goh[:, :, g:g + 1].to_broadcast([128, NT, 8]), op=ALU.mult), r=[brt], w=[B("comb")])

    S.barrier()
    if stop == "C":
        finish([("x1", x1[:], [128, NT, D], F32), ("h2T", h2T[:], [128, 8, SEQ], BF16), ("comb", comb[:], [128, NT, 32], F32),
                ("logits", logits[:], [128, NT, 36], F32)])
        return

    L.off = Lkeep
    s_t2 = [L.alloc(f"s_t{i}", [128, 512], F32) for i in range(2)]
    actT2 = [L.alloc(f"actT{i}", [128, 2, 512], BF16) for i in range(2)]
    units = [(e_, g) for e_ in range(NEXP) for g in range(4)]
    NU = len(units)
    out_toks = []

    def u_up(q, fcs=(0, 1)):
        e_, g = units[q]
        ws = e_ % 2
        aT = actT2[q % 2]
        baT = B("actT2", q % 2)
        cs = slice(g * 512, (g + 1) * 512)
        rdh = [B("h2T", 4 * g + j) for j in range(4)]
        for fc in fcs:
            hsel = (2 * q + fc) % 2
            k1, k3 = 2 * hsel, 2 * hsel + 1
            for dc in range(8):
                T(lambda e, dc=dc: e.matmul(bank(k1), lhsT=W13[ws][:, dc, fc * 128:(fc + 1) * 128], rhs=h2T[:, dc, cs], start=(dc == 0), stop=(dc == 7)),
                  r=rdh + [B("W13", ws)], w=[B("bk", k1)])
            for dc in range(8):
                T(lambda e, dc=dc: e.matmul(bank(k3), lhsT=W13[ws][:, dc, 256 + fc * 128:256 + (fc + 1) * 128], rhs=h2T[:, dc, cs], start=(dc == 0), stop=(dc == 7)),
                  r=rdh + [B("W13", ws)], w=[B("bk", k3)])
            st_ = s_t2[hsel]
            A(lambda e: e.activation(out=st_[:], in_=bank(k1), func=AF.Silu), r=[B("bk", k1)], w=[B("s_t2", hsel)])
            V(lambda e: e.tensor_tensor(out=aT[:, fc, :], in0=bank(k3), in1=st_[:], op=ALU.mult), r=[B("bk", k3), B("s_t2", hsel)], w=[baT])

    dn_ctr = [0]

    def u_down(q, js=(0, 1, 2, 3)):
        e_, g = units[q]
        ws = e_ % 2
        aT = actT2[q % 2]
        baT = B("actT2", q % 2)
        for j in js:
            i = 4 * g + j
            d_ = dn_ctr[0] % 2
            dn_ctr[0] += 1
            pd = PS[2 + d_]
            bpd = B("PSU", 2 + d_)
            for hf in range(2):
                for fc in range(2):
                    T(lambda e, hf=hf, fc=fc: e.matmul(pd[:, hf * 512:(hf + 1) * 512], lhsT=aT[:, fc, j * 128:(j + 1) * 128], rhs=W2b[ws][:, fc, hf * 512:(hf + 1) * 512], start=(fc == 0), stop=(fc == 1)),
                      r=[baT, B("W2b", ws)], w=[bpd])
            bx1 = B("x1", i)
            V(lambda e, i=i: e.scalar_tensor_tensor(out=x1[:, i, :], in0=pd[:, :], scalar=comb[:, i, e_:e_ + 1], in1=x1[:, i, :], op0=ALU.mult, op1=ALU.add),
              r=[bpd, B("comb"), bx1], w=[bx1])
            if e_ == NEXP - 1:
                out_toks.append(S.dma("pool", out_d[i * 128:(i + 1) * 128, :], x1[:, i, :], reads=[bx1]))

    for q in range(NU + 1):
        if q < NU:
            e_, g = units[q]
            if g == 1 and e_ + 1 < NEXP:
                moe_load(e_ + 1)
            u_up(q, (0,))
            if q >= 1:
                u_down(q - 1, (0, 1))
            u_up(q, (1,))
            if q >= 1:
                u_down(q - 1, (2, 3))
        else:
            u_down(q - 1)

    for t in out_toks:
        S._wait("pool", t)
    S.barrier()


_CACHE = {}


def _consts():
    s = np.arange(SEQ)
    ktab = np.stack([128.0 * (s // 128), (s % 128).astype(np.float64), np.ones(SEQ)]).astype(np.float32)
    slopes = 2.0 ** (-(np.arange(1, 9)).astype(np.float64))
    qtab = np.zeros((3, 8, SEQ), np.float32)
    for h in range(8):
        qtab[0, h] = slopes[h]
        qtab[1, h] = slopes[h]
        qtab[2, h] = -slopes[h] * (128.0 * (s // 128) + 64.0)
    sl = np.arange(128)
    rel = np.maximum(sl[:, None] - sl[None, :], 0).astype(np.float64)
    dmat = np.zeros((128, 8, 128), np.float32)
    for h in range(8):
        dmat[:, h, :] = -2.0 * slopes[h] * rel
    bf = ml_dtypes.bfloat16
    return dict(ktab=ktab.astype(bf), qtab=qtab.astype(bf), dmat=dmat.astype(bf),
                identb=np.eye(128, dtype=np.float32).astype(bf), identf=np.eye(128, dtype=np.float32))


def kernel(x, c, ada_w, ada_b, norm_mix_w, w_in, q_norm_w, k_norm_w, conv_w, conv_b,
           w_rec_gate, b_rec_gate, w_in_gate, b_in_gate, lru_lambda, w_proj_a, w_proj_b,
           w_out, norm_ffn_w, w_group, b_group, w_expert_router, b_expert_router, w1, w3, w2):
    f = lambda a: np.ascontiguousarray(np.asarray(a, dtype=np.float32))
    col = lambda v: f(v).reshape(-1, 128).T
    x = f(x); c = f(c)
    ab = f(ada_b)[0]
    cw = f(conv_w)[0]
    shared = [col(norm_mix_w[0]), col(norm_ffn_w[0]), col(ab[0:D]), col(ab[D:2 * D]), col(ab[3 * D:4 * D]), col(ab[4 * D:5 * D])]
    shared += [col(cw[j]) for j in range(4)]
    shared += [col(f(conv_b)[0]), col(f(b_rec_gate)[0]), col(f(b_in_gate)[0]), col(f(lru_lambda)[0])]
    shared = np.concatenate(shared, axis=1)
    rows = np.concatenate([f(b_group)[0], f(b_expert_router)[0]])[None, :].repeat(128, axis=0)
    gbias = np.concatenate([ab[2 * D:3 * D], ab[5 * D:6 * D]])[None, :].repeat(128, axis=0)
    qkw = np.concatenate([np.tile(f(q_norm_w)[0], 8), f(k_norm_w)[0]])[None, :].repeat(128, axis=0)
    wbd = np.zeros((128, 2, 4, 128), np.float32)
    for g, wsrc in enumerate((f(w_rec_gate)[0], f(w_in_gate)[0])):
        for cc in range(4):
            for bl in range(2):
                wbd[bl * 64:(bl + 1) * 64, g, cc, bl * 64:(bl + 1) * 64] = wsrc[2 * cc + bl]
    w_rt = np.concatenate([f(w_group)[0], f(w_expert_router)[0]], axis=1)
    common = dict(rows=f(rows), gbias=f(gbias), qkw=f(qkw), ada_w=f(ada_w)[0], w_in=f(w_in)[0], w_proj_a=f(w_proj_a)[0], w_proj_b=f(w_proj_b)[0],
                  w_out=f(w_out)[0], w_rt=f(w_rt), wbd=wbd, w1=f(w1)[0], w3=f(w3)[0], w2=f(w2)[0])
    common.update(_consts())
    in_maps = []
    for b in range(8):
        m = dict(common)
        m["x"] = x[b]
        m["cols"] = np.ascontiguousarray(np.concatenate([col(c[b]), shared], axis=1))
        in_maps.append(m)
    if "nc" not in _CACHE:
        _CACHE["nc"] = build_program()
    res = run_bass_kernel_spmd(_CACHE["nc"], in_maps, core_ids=list(range(8)))
    return np.stack([np.asarray(r["out"], dtype=np.float32) for r in res.results], axis=0)
```
